# Optimizing a Trainium2 kernel written in Bass

```python
import jax, jax.numpy as jnp
from jax import lax
import numpy as np

D_MODEL = 2048
BATCH = 2
SEQ = 8192
DEPTH = 1

HEAD_DIM = 64
ATTN_WIDTH = D_MODEL // 2
ATTN_HEADS = ATTN_WIDTH // HEAD_DIM
ATTN_KV_HEADS = ATTN_HEADS // 4
KV_WIDTH = ATTN_KV_HEADS * HEAD_DIM
WINDOW = 128
BLOCK = 128

POOL_WINDOWS = (2, 4, 8, 16)
POOL_GROUPS = len(POOL_WINDOWS)
POOL_WIDTH = D_MODEL // 2
POOL_GROUP_DIM = POOL_WIDTH // POOL_GROUPS

N_BRANCHES = 2
Q_END = ATTN_WIDTH
K_END = Q_END + KV_WIDTH
V_END = K_END + KV_WIDTH
U_END = V_END + POOL_WIDTH
IN_WIDTH = U_END + N_BRANCHES * D_MODEL

N_GROUPS = 4
EXPERTS_PER_GROUP = 4
N_EXPERTS = N_GROUPS * EXPERTS_PER_GROUP
TOP_K = 2
EXPERT_FF = D_MODEL // 4

N_ADA = 6
NORM_EPS = 1e-6
MASK_VALUE = -1e30

kernel_name = "hybrid_swa_sink_pool_hmoe_adaln"


def rms_norm(x, g):
    xf = x.astype(jnp.float32)
    xf = xf * lax.rsqrt(jnp.mean(xf * xf, axis=-1, keepdims=True) + NORM_EPS)
    return (xf * g.astype(jnp.float32)).astype(x.dtype)


def sliding_window_sink_attention(q, k, v, sinks):
    b, s = q.shape[0], q.shape[1]
    nb = s // BLOCK
    grp = ATTN_HEADS // ATTN_KV_HEADS
    qb = q.reshape(b, nb, BLOCK, ATTN_KV_HEADS, grp, HEAD_DIM)
    pad = jnp.zeros((b, BLOCK, ATTN_KV_HEADS, HEAD_DIM), k.dtype)
    kp = jnp.concatenate([pad, k], axis=1).reshape(b, nb + 1, BLOCK, ATTN_KV_HEADS, HEAD_DIM)
    vp = jnp.concatenate([pad, v], axis=1).reshape(b, nb + 1, BLOCK, ATTN_KV_HEADS, HEAD_DIM)
    k_band = jnp.concatenate([kp[:, :-1], kp[:, 1:]], axis=2)
    v_band = jnp.concatenate([vp[:, :-1], vp[:, 1:]], axis=2)
    logits = jnp.einsum('bnqkgd,bnjkd->bnkgqj', qb, k_band,
                        preferred_element_type=jnp.float32) * (HEAD_DIM ** -0.5)
    r = jnp.arange(BLOCK)[:, None]
    j = jnp.arange(2 * BLOCK)[None, :]
    rel = r + BLOCK - j
    band = (rel >= 0) & (rel < WINDOW)
    blk = jnp.arange(nb)[:, None, None]
    valid = band[None] & ((blk > 0) | (j[None] >= BLOCK))
    logits = jnp.where(valid[None, :, None, None], logits, MASK_VALUE)
    sink = jnp.broadcast_to(
        sinks.astype(jnp.float32).reshape(ATTN_KV_HEADS, grp)[None, None, :, :, None, None],
        logits.shape[:-1] + (1,))
    probs = jax.nn.softmax(jnp.concatenate([logits, sink], axis=-1), axis=-1)[..., :-1]
    out = jnp.einsum('bnkgqj,bnjkd->bnqkgd', probs.astype(v.dtype), v_band)
    return out.reshape(b, s, ATTN_WIDTH)


def multiscale_pool(u, w_pool, pool_scale):
    b, s = u.shape[0], u.shape[1]
    ug = u.reshape(b, s, POOL_GROUPS, POOL_GROUP_DIM)
    pos = jnp.arange(s)
    outs = []
    for gi, w in enumerate(POOL_WINDOWS):
        xg = ug[:, :, gi].astype(jnp.float32)
        cs = jnp.cumsum(xg, axis=1)
        cs_prev = jnp.pad(cs, ((0, 0), (w, 0), (0, 0)))[:, :s]
        count = jnp.minimum(pos + 1, w).astype(jnp.float32)[None, :, None]
        outs.append((cs - cs_prev) / count - xg)
    pooled = jnp.stack(outs, axis=2).astype(u.dtype)
    mixed = jnp.einsum('bsgc,gcd->bsgd', pooled, w_pool).reshape(b, s, POOL_WIDTH)
    return mixed * pool_scale


def hierarchical_moe(h, w_router_group, b_router_group, w_router_expert, b_router_expert,
                     w_e_gate, w_e_up, w_e_down):
    t = h.shape[0]
    g_logits = jnp.matmul(h, w_router_group).astype(jnp.float32) + b_router_group.astype(jnp.float32)
    g_prob = jax.nn.softmax(g_logits, axis=-1)
    g_top_p, g_idx = lax.top_k(g_prob, 1)
    e_logits = (jnp.matmul(h, w_router_expert).astype(jnp.float32)
                + b_router_expert.astype(jnp.float32)).reshape(t, N_GROUPS, EXPERTS_PER_GROUP)
    e_sel = jnp.take_along_axis(e_logits, g_idx[:, :, None], axis=1)[:, 0]
    e_prob = jax.nn.softmax(e_sel, axis=-1)
    e_top_p, e_idx = lax.top_k(e_prob, TOP_K)
    e_top_p = e_top_p / jnp.sum(e_top_p, axis=-1, keepdims=True)
    weights = g_top_p * e_top_p
    global_idx = g_idx * EXPERTS_PER_GROUP + e_idx
    combine = jnp.sum(jax.nn.one_hot(global_idx, N_EXPERTS, dtype=jnp.float32)
                      * weights[..., None], axis=1)
    gate = jnp.einsum('td,edf->tef', h, w_e_gate)
    up = jnp.einsum('td,edf->tef', h, w_e_up)
    act = jax.nn.silu(gate) * up * combine[:, :, None].astype(h.dtype)
    return jnp.einsum('tef,efd->td', act, w_e_down)


def setup_inputs(seed: int = 0) -> dict:
    key = jax.random.key(seed)
    ks = jax.random.split(key, 22)
    n = jax.random.normal
    f32 = jnp.float32
    L, D = DEPTH, D_MODEL
    return {
        "x": n(ks[0], (BATCH, SEQ, D), f32),
        "c": n(ks[1], (BATCH, D), f32),
        "w_ada": n(ks[2], (L, D, N_ADA * D), f32) * (0.5 * D ** -0.5),
        "b_ada": n(ks[3], (L, N_ADA * D), f32) * 0.02,
        "norm1_g": 1.0 + 0.02 * n(ks[4], (L, D), f32),
        "w_in": n(ks[5], (L, D, IN_WIDTH), f32) * D ** -0.5,
        "sinks": n(ks[6], (L, ATTN_HEADS), f32),
        "w_pool": n(ks[7], (L, POOL_GROUPS, POOL_GROUP_DIM, POOL_GROUP_DIM), f32) * POOL_GROUP_DIM ** -0.5,
        "pool_scale": 1.0 + 0.1 * n(ks[8], (L, POOL_WIDTH), f32),
        "w_attn_branch": n(ks[9], (L, ATTN_WIDTH, D), f32) * ATTN_WIDTH ** -0.5,
        "w_pool_branch": n(ks[10], (L, POOL_WIDTH, D), f32) * POOL_WIDTH ** -0.5,
        "w_out": n(ks[11], (L, D, D), f32) * D ** -0.5,
        "norm2_g": 1.0 + 0.02 * n(ks[12], (L, D), f32),
        "w_router_group": n(ks[13], (L, D, N_GROUPS), f32) * D ** -0.5,
        "b_router_group": n(ks[14], (L, N_GROUPS), f32) * 0.01,
        "w_router_expert": n(ks[15], (L, D, N_EXPERTS), f32) * D ** -0.5,
        "b_router_expert": n(ks[16], (L, N_EXPERTS), f32) * 0.01,
        "w_e_gate": n(ks[17], (L, N_EXPERTS, D, EXPERT_FF), f32) * D ** -0.5,
        "w_e_up": n(ks[18], (L, N_EXPERTS, D, EXPERT_FF), f32) * D ** -0.5,
        "w_e_down": n(ks[19], (L, N_EXPERTS, EXPERT_FF, D), f32) * EXPERT_FF ** -0.5,
        "final_g": 1.0 + 0.02 * n(ks[20], (D,), f32),
    }


def reference(x, c, w_ada, b_ada, norm1_g, w_in, sinks, w_pool, pool_scale, w_attn_branch,
              w_pool_branch, w_out, norm2_g, w_router_group, b_router_group, w_router_expert,
              b_router_expert, w_e_gate, w_e_up, w_e_down, final_g):
    b, s, d = x.shape
    for l in range(DEPTH):
        ada = (jnp.matmul(c, w_ada[l]) + b_ada[l])[:, None, :]
        shift1, scale1, gate1, shift2, scale2, gate2 = jnp.split(ada, N_ADA, axis=-1)

        h = rms_norm(x, norm1_g[l]) * (1.0 + scale1) + shift1
        proj = jnp.matmul(h, w_in[l])
        q, k, v, u, gate_logits = jnp.split(proj, [Q_END, K_END, V_END, U_END], axis=-1)
        attn = sliding_window_sink_attention(
            q.reshape(b, s, ATTN_HEADS, HEAD_DIM),
            k.reshape(b, s, ATTN_KV_HEADS, HEAD_DIM),
            v.reshape(b, s, ATTN_KV_HEADS, HEAD_DIM),
            sinks[l])
        pool = multiscale_pool(u, w_pool[l], pool_scale[l])
        gate_a, gate_p = jnp.split(jax.nn.sigmoid(gate_logits), N_BRANCHES, axis=-1)
        merged = (gate_a * jnp.matmul(attn, w_attn_branch[l])
                  + gate_p * jnp.matmul(pool, w_pool_branch[l]))
        x = x + gate1 * jnp.matmul(merged, w_out[l])

        h2 = rms_norm(x, norm2_g[l]) * (1.0 + scale2) + shift2
        y = hierarchical_moe(h2.reshape(b * s, d), w_router_group[l], b_router_group[l],
                             w_router_expert[l], b_router_expert[l],
                             w_e_gate[l], w_e_up[l], w_e_down[l]).reshape(b, s, d)
        x = x + gate2 * y
    return rms_norm(x, final_g)
```

```python
import numpy as np
from contextlib import ExitStack
import concourse.bass as bass
import concourse.mybir as mybir
from concourse.bass_utils import run_bass_kernel_spmd

F32 = mybir.dt.float32
BF16 = mybir.dt.bfloat16
U8 = mybir.dt.uint8
AF = mybir.ActivationFunctionType
ALU = mybir.AluOpType
AX = mybir.AxisListType

D = 2048
NCORE = 8
NTOK = 2048
TB = 512
NB = NTOK // TB
HT = TB + 128
NEXP = 16
EPS = 1e-6
NEG = -1e30

C_C, C_BADA, C_G1, C_SINK, C_PSC, C_G2, C_BR, C_FG, C_UF, C_INVC, C_AM, C_AM0, C_WR = (
    0, 16, 112, 128, 144, 152, 168, 188, 204, 205, 269, 525, 781)
C_TOT = 781 + 320


class T:
    __slots__ = ("w", "r")

    def __init__(self):
        self.w = None
        self.r = {}


class Prog:
    ENGS = ("pe", "act", "dve", "pool", "sp")

    def __init__(self):
        self.ops = {e: [] for e in self.ENGS}
        self.seen = {e: {} for e in self.ENGS}
        self.dma_cnt = {}
        self.need = {e: set() for e in self.ENGS}

    def op(self, eng, fn, reads=(), writes=(), dma=None, same_ok=False):
        deps = {}

        def add(k, v):
            if deps.get(k, -1) < v:
                deps[k] = v

        for t in reads:
            if t.w is not None:
                add(t.w[:2], t.w[2])
        for t in writes:
            if t.w is not None:
                add(t.w[:2], t.w[2])
            for k, v in t.r.items():
                add(k, v)
        idx = len(self.ops[eng])
        waits = []
        sn = self.seen[eng]
        for k, val in deps.items():
            if k[0] == "e" and k[1] == eng and (eng == "pe" or same_ok):
                continue
            if sn.get(k, -1) >= val:
                continue
            sn[k] = val
            waits.append((k[0], k[1], val))
            if k[0] == "e":
                self.need[k[1]].add(val)
        if dma is not None:
            self.dma_cnt[dma] = self.dma_cnt.get(dma, 0) + 16
            me = ("d", dma, self.dma_cnt[dma])
        else:
            me = ("e", eng, idx)
        self.ops[eng].append((waits, fn, dma))
        for t in reads:
            if t.r.get(me[:2], -1) < me[2]:
                t.r[me[:2]] = me[2]
        for t in writes:
            t.w = me
            t.r = {}
        return me

    def fence(self, new, old):
        deps = {}
        for t in old:
            if t.w is not None and deps.get(t.w[:2], -1) < t.w[2]:
                deps[t.w[:2]] = t.w[2]
            for k, v in t.r.items():
                if deps.get(k, -1) < v:
                    deps[k] = v
        for t in new:
            t.w = None
            t.r = dict(deps)

    def emit(self, nc, sems, dma_sems, final_waits):
        rank = {}
        for e in self.ENGS:
            s = sorted(self.need[e])
            rank[e] = {v: i + 1 for i, v in enumerate(s)}
        handles = {"pe": "tensor", "act": "scalar", "dve": "vector", "pool": "gpsimd", "sp": "sync"}

        def replay(engname, e):
            rk = rank[engname]
            for idx, (waits, fn, dma) in enumerate(self.ops[engname]):
                for kind, key, val in waits:
                    if kind == "e":
                        e.wait_ge(sems[key], rank[key][val])
                    else:
                        e.wait_ge(dma_sems[key], val)
                ins = fn(e)
                if dma is not None:
                    ins.then_inc(dma_sems[dma], 16)
                elif idx in rk:
                    ins.then_inc(sems[engname], 1)
            if engname == "sp":
                for key in final_waits:
                    e.wait_ge(dma_sems[key], self.dma_cnt[key])

        with nc.Block() as block:
            for engname in self.ENGS:
                getattr(block, handles[engname])(lambda e, n=engname: replay(n, e))


ADA_PRO = 16
HOIST_AT = 6


def build_nc():
    nc = bass.Bass("TRN2", target_bir_lowering=False)

    def din(name, shape):
        return nc.dram_tensor(name, shape, F32, kind="ExternalInput").ap()

    x_d = din("x", [NTOK + 128, D])
    cst_d = din("cst", [128, C_TOT])
    w_ada = din("w_ada", [D, 6 * D])
    w_in = din("w_in", [D, 6656])
    w_pool = din("w_pool", [4, 256, 256])
    w_ab = din("w_ab", [1024, D])
    w_pb = din("w_pb", [1024, D])
    w_out = din("w_out", [D, D])
    w_eg = din("w_eg", [NEXP, D, 512])
    w_eu = din("w_eu", [NEXP, D, 512])
    w_ed = din("w_ed", [NEXP, 512, D])
    fg_d = din("fg_bc", [128, D])
    y_d = nc.dram_tensor("y", [NTOK, D], F32, kind="ExternalOutput").ap()

    P = Prog()
    with ExitStack() as st:
        def sb(name, shape, dt):
            return st.enter_context(nc.sbuf_tensor(name, shape, dt))

        NSLOT = 6
        SLOTE = 4096
        wring = [sb("wring%d" % i, [128, SLOTE], BF16) for i in range(NSLOT)]
        xacc = sb("xacc", [128, 4, D], F32)
        bc = sb("bc", [128, D], F32)
        cst = sb("cst_sb", [128, C_TOT], F32)
        ident_f = sb("ident_f", [128, 128], F32)
        ones_f = sb("ones_f", [128, 128], F32)
        ident_b = sb("ident_b", [128, 128], BF16)
        amask_b = sb("amask_b", [128, 512], BF16)
        ada_fm = sb("ada_fm", [128, 96], F32)
        a12 = sb("a12", [128, 32], F32)
        c_bf = sb("c_bf", [128, 16], BF16)
        stt_ = sb("stats", [128, 64], F32)
        stA = [sb("stA%d" % i, [128, 32], F32) for i in range(2)]
        dg = [sb("dg%d" % i, [128, 128], F32) for i in range(2)]
        dgrs = [sb("dgr%d" % i, [128, 128], F32) for i in range(2)]
        comb = sb("comb", [128, 4, 16], F32)
        rts = [sb("rt%d" % i, [128, 96], F32) for i in range(2)]
        OFFA, OFFB, OFFC = 0, 20480, 36864
        PPB = OFFC + 69760
        pp = sb("pp", [128, PPB], U8)
        ps = st.enter_context(nc.psum_tensor("ps", [128, 4096], F32))

        def view(off, n, dt, pat=None, **kw):
            sz = 2 if dt == BF16 else 4
            v = pp[:, off:off + n * sz].bitcast(dt)
            if pat:
                v = v.rearrange(pat, **kw)
            return v

        K3 = "p (k t) -> p k t"
        hT = view(OFFA, 16 * HT, BF16, K3, k=16)
        attnT = view(OFFB, 8 * TB, BF16, K3, k=8)
        pmixT = view(OFFB + 8192, 8 * TB, BF16, K3, k=8)
        actb = [view(OFFB + i * 4096, 4 * TB, BF16, K3, k=4) for i in range(2)]
        stmp = [view(OFFB + 8192 + i * 2048, 512, F32) for i in range(4)]
        xnj = [view(OFFC + i * 4096, D, BF16) for i in range(2)]
        xt = [view(OFFC + 8192 + i * 8192, D, F32) for i in range(2)]
        qT = view(OFFC + 24576, 8 * TB, BF16, K3, k=8)
        kT2 = view(OFFC + 32768, 4 * HT, BF16, K3, k=4)
        Vt = view(OFFC + 37888, 5 * 512, BF16, "p (t g d) -> p t g d", t=5, g=4)
        ub = [view(OFFC + 43008 + i * 2176, 544, F32) for i in range(2)]
        sAB = [view(OFFC + 47360 + i * 2176, 544, F32) for i in range(2)]
        pooled = view(OFFC + 51712, 8 * TB, BF16, K3, k=8)
        Pb = [view(OFFC + 59904 + i * 2048, 4 * 256, BF16, K3, k=4) for i in range(2)]
        PTs = [view(OFFC + 64000 + i * 2048, 1024, BF16) for i in range(2)]
        siga = view(OFFC, 16 * TB, BF16, K3, k=16)
        sigp_lo = view(OFFC + 16384, 8 * TB, BF16, K3, k=8)
        sigp_hi = view(OFFC + 43008, 8 * TB, BF16, K3, k=8)
        mergedT = view(OFFC + 24576, 16 * TB, BF16, K3, k=16)
        tA = [view(OFFC + 59904 + i * 2048, 512, F32) for i in range(2)]
        tB = [view(OFFC + 64000 + i * 2048, 512, F32) for i in range(2)]
        xn2j = [view(OFFC + i * 4096, D, BF16) for i in range(2)]
        h2Tf = [view(OFFC + 8192 + i * 8192, 16 * 128, F32, K3, k=16) for i in range(2)]
        sl = [view(OFFC + 24576 + i * 2048, 512, F32) for i in range(2)]
        h2T = view(OFFC + 28672, 16 * TB, BF16, K3, k=16)
        fjunk = view(OFFC + 45056, D, BF16)
        bc2 = view(OFFC + 49152, D, F32)

        sems = {e: st.enter_context(nc.semaphore("s_" + e)) for e in Prog.ENGS}
        dnames = ["w%d" % i for i in range(NSLOT)] + ["xt0", "xt1", "cst", "kdup", "bc2", "bcs", "bcl"] + \
                 ["xa%d" % i for i in range(4)] + ["out%d" % i for i in range(4)]
        dsem = {k: st.enter_context(nc.semaphore("d_" + k)) for k in dnames}

        t_w = [T() for _ in range(NSLOT)]
        t_pb = [T() for _ in range(8)]
        t_cst, t_id, t_ada, t_a12, t_cbf, t_bc, t_comb, t_ada2, t_a2, t_ada2a = [T() for _ in range(10)]
        t_dg = [T(), T()]
        t_xa = [T() for _ in range(4)]
        t_hT = [T() for _ in range(5)]
        t_h2T = [T() for _ in range(4)]
        t_attn = [T() for _ in range(4)]
        t_pmix = T()
        t_act = [T(), T()]
        t_stmp = [T() for _ in range(4)]
        t_xnj, t_xt = [T(), T()], [T(), T()]
        t_dgr = [T(), T()]
        t_q, t_kn, t_kd, t_v = T(), T(), T(), T()
        t_ub, t_s = [T(), T()], [T(), T()]
        t_pooled = [T() for _ in range(8)]
        t_P, t_PT = [T(), T()], [T(), T()]
        t_sa = [T(), T()]
        t_merged = [T() for _ in range(4)]
        t_siga, t_sigp = [T() for _ in range(4)], [T() for _ in range(4)]
        t_tA, t_tB = [T(), T()], [T(), T()]
        t_xn2j, t_h2Tf, t_sl = [T(), T()], [T(), T()], [T(), T()]
        t_st = [T() for _ in range(8)]
        t_rts = [T(), T()]
        regA_M = t_hT
        regA_E = []
        regB_M = t_attn + [t_pmix]
        regB_E = t_act + t_stmp
        regC_M1_rest = [t_q, t_kn, t_kd, t_v] + t_ub + t_s + t_pooled + t_P + t_PT
        regC_M1 = t_xnj + t_xt + regC_M1_rest
        regC_M2 = t_merged + t_siga + t_sigp + t_tA + t_tB
        t_fj, t_bc2 = T(), T()
        t_gsc = [T(), T()]
        regC_E = t_xn2j + t_h2Tf + t_sl + t_h2T + [t_fj, t_bc2]

        state = {"bank": 0, "ev": 0, "resv": set(), "live": set()}

        def bank(n=1):
            b0 = state["bank"]
            for off in range(0, 16):
                b = (b0 + off) % 8
                if n > 1 and b % n:
                    continue
                if b + n > 8:
                    continue
                if any((b + i) in state["resv"] or (b + i) in state["live"] for i in range(n)):
                    continue
                for i in range(n):
                    state["live"].add(b + i)
                state["bank"] = (b + n) % 8
                return b
            raise AssertionError("out of PSUM banks: live=%s resv=%s" % (state["live"], state["resv"]))

        def bfree(b, n=1):
            for i in range(n):
                state["live"].discard(b + i)

        def pbank(b, n=1, c0=0, c1=None):
            if c1 is None:
                c1 = 512 * n
            return ps[:, b * 512 + c0:b * 512 + c1]

        def cs(c0, n):
            return cst[:, c0:c0 + n]

        def kcols(w2d, c0, ncols, k0, nk):
            src = w2d.rearrange("(k p) c -> p k c", p=128)[:, k0:k0 + nk, c0:c0 + ncols]
            return [(lambda sl_: sl_[:, 0:nk * ncols].rearrange(K3, k=nk), src)]

        def all_loads():
            for i in range(ADA_PRO):
                yield ("ada", kcols(w_ada, i * 256, 256, 0, 16))
            ada_i = [ADA_PRO]

            def ada_more(n=1):
                out = []
                for _ in range(n):
                    if ada_i[0] < 48:
                        out.append(("ada", kcols(w_ada, ada_i[0] * 256, 256, 0, 16)))
                        ada_i[0] += 1
                return out

            for blk in range(NB):
                for i in range(4):
                    yield ("q", kcols(w_in, i * 256, 256, 0, 16))
                yield ("k", kcols(w_in, 1024, 256, 0, 16))
                yield ("v", kcols(w_in, 1280, 256, 0, 16))
                for i in range(4):
                    yield ("u", kcols(w_in, 1536 + i * 256, 256, 0, 16))
                for gi in range(16):
                    yield ("g", kcols(w_in, 2560 + (gi // 8) * 2048 + (gi % 8) * 256, 256, 0, 16))
                    yield from ada_more()
                yield ("wp", [(lambda sl_: sl_[:, 0:2048].rearrange("p (g k d) -> p g k d", g=4, k=2),
                               w_pool.rearrange("g (k p) d -> p g k d", p=128))])
                yield from ada_more()
                for og in range(4):
                    yield ("brA", kcols(w_ab, og * 512, 512, 0, 8))
                    yield ("brB", kcols(w_pb, og * 512, 512, 0, 8))
                    yield from ada_more(2)
                for db in range(4):
                    for kh in range(2):
                        yield ("wo", kcols(w_out, db * 512, 512, kh * 8, 8))
                        yield from ada_more()
                for ex in range(NEXP):
                    for hh in range(2):
                        yield ("eg", kcols(w_eg[ex], hh * 256, 256, 0, 16))
                        yield ("eu", kcols(w_eu[ex], hh * 256, 256, 0, 16))
                    for dh in range(2):
                        yield ("ed", kcols(w_ed[ex], dh * 1024, 1024, 0, 4))

        specs = list(all_loads())
        wq = {"issued": 0, "released": 0, "acq": 0}

        def pump():
            while wq["issued"] < len(specs) and wq["issued"] < wq["released"] + NSLOT:
                i = wq["issued"]
                s = i % NSLOT
                for dfn, src in specs[i][1]:
                    d = dfn(wring[s])
                    P.op("pool", lambda e, d=d, src=src: e.dma_start(out=d, in_=src), writes=[t_w[s]],
                         dma="w%d" % s)
                wq["issued"] += 1

        def wnext(tag):
            i = wq["acq"]
            assert specs[i][0] == tag, (specs[i][0], tag, i)
            pump()
            assert wq["issued"] > i, "weight queue: too many held"
            wq["acq"] += 1
            return i % NSLOT

        def wrel(n=1):
            wq["released"] += n
            pump()

        def slot3(s, k):
            return wring[s][:, :].rearrange(K3, k=k)

        def evac(out_ap, in_ap, reads, writes, scale=None, same_ok=True):
            state["ev"] ^= 1
            if state["ev"]:
                if scale is None:
                    P.op("act", lambda e: e.activation(out=out_ap, in_=in_ap, func=AF.Copy),
                         reads=reads, writes=writes, same_ok=same_ok)
                else:
                    P.op("act", lambda e: e.activation(out=out_ap, in_=in_ap, func=AF.Copy, scale=scale),
                         reads=reads, writes=writes, same_ok=same_ok)
            else:
                if scale is None:
                    P.op("dve", lambda e: e.tensor_copy(out=out_ap, in_=in_ap),
                         reads=reads, writes=writes, same_ok=same_ok)
                else:
                    P.op("dve", lambda e: e.tensor_scalar(out=out_ap, in0=in_ap, scalar1=scale, scalar2=None,
                                                         op0=ALU.mult),
                         reads=reads, writes=writes, same_ok=same_ok)

        def mm(out_ap, pairs, reads, btiles, start=True, stop=True):
            n = len(pairs)
            for i, (l, r) in enumerate(pairs):
                P.op("pe", lambda e, l=l, r=r, i=i: e.matmul(out_ap, lhsT=l, rhs=r, start=(start and i == 0),
                                                            stop=(stop and i == n - 1)),
                     reads=reads, writes=btiles)

        P.op("sp", lambda e: e.dma_start(out=cst[:, :], in_=cst_d), writes=[t_cst], dma="cst")
        pump()
        P.op("pool", lambda e: e.memset(ident_f[:, :], 0.0), writes=[t_id])
        P.op("pool", lambda e: e.affine_select(out=ident_f[:, :], in_=ident_f[:, :], pattern=[[-1, 128]],
                                              compare_op=ALU.not_equal, fill=1.0, base=0,
                                              channel_multiplier=1), reads=[t_id], writes=[t_id])
        P.op("pool", lambda e: e.memset(ones_f[:, :], 1.0), writes=[t_id], same_ok=True)
        cs_eps = stt_[:, 28:29]
        P.op("pool", lambda e: e.memset(stt_[:, 28:29], EPS), writes=[t_id], same_ok=True)
        P.op("dve", lambda e: e.tensor_copy(out=c_bf[:, :], in_=cs(C_C, 16)), reads=[t_cst], writes=[t_cbf])
        P.op("dve", lambda e: e.tensor_copy(out=ident_b[:, :], in_=ident_f[:, :]), reads=[t_id], writes=[t_id])
        P.op("dve", lambda e: e.tensor_copy(out=amask_b[:, :], in_=cs(C_AM, 512)), reads=[t_cst, t_id],
             writes=[t_id])

        bA = bank()
        bfree(bA)
        state["resv"].add(bA)
        ada_n = [0]

        def ada_consume(n=1):
            for _ in range(n):
                if ada_n[0] >= 48:
                    return
                s_i = ada_n[0]
                ada_n[0] += 1
                s = wnext("ada")
                w3 = slot3(s, 16)
                for nn in range(2):
                    j = s_i * 2 + nn
                    mm(ps[:, bA * 512 + j:bA * 512 + j + 1],
                       [(w3[:, k, nn * 128:(nn + 1) * 128], c_bf[:, k:k + 1]) for k in range(16)],
                       [t_w[s], t_cbf], [t_pb[bA]])
                wrel()

        ada_consume(ADA_PRO)
        P.op("dve", lambda e: e.tensor_tensor(out=ada_fm[:, 0:32], in0=ps[:, bA * 512:bA * 512 + 32],
                                             in1=cs(C_BADA, 32), op=ALU.add),
             reads=[t_pb[bA], t_cst], writes=[t_ada])
        P.op("dve", lambda e: e.scalar_tensor_tensor(out=a12[:, 0:16], in0=ada_fm[:, 16:32], scalar=1.0,
                                                    in1=cs(C_G1, 16), op0=ALU.add, op1=ALU.mult),
             reads=[t_ada, t_cst], writes=[t_a12])

        def ada_finish_a():
            assert ada_n[0] >= 24
            P.op("dve", lambda e: e.tensor_tensor(out=ada_fm[:, 32:48], in0=ps[:, bA * 512 + 32:bA * 512 + 48],
                                                 in1=cs(C_BADA + 32, 16), op=ALU.add),
                 reads=[t_pb[bA], t_cst], writes=[t_ada2a])

        def ada_finish():
            assert ada_n[0] == 48
            P.op("dve", lambda e: e.tensor_tensor(out=ada_fm[:, 48:96], in0=ps[:, bA * 512 + 48:bA * 512 + 96],
                                                 in1=cs(C_BADA + 48, 48), op=ALU.add),
                 reads=[t_pb[bA], t_cst], writes=[t_ada2])
            P.op("dve", lambda e: e.scalar_tensor_tensor(out=a12[:, 16:32], in0=ada_fm[:, 64:80], scalar=1.0,
                                                        in1=cs(C_G2, 16), op0=ALU.add, op1=ALU.mult),
                 reads=[t_ada2, t_cst], writes=[t_a2])
            state["resv"].discard(bA)

        def regen_bc(vec_ap, vec_tiles, factor=1.0, dst=None, tdst=None):
            dst = bc if dst is None else dst
            tdst = t_bc if tdst is None else tdst
            for j4 in range(4):
                b = bank()
                for jj in range(4):
                    j = j4 * 4 + jj
                    P.op("dve", lambda e, j=j: e.tensor_scalar(out=dg[j % 2][:, :], in0=ident_f[:, :],
                                                              scalar1=vec_ap[:, j:j + 1], scalar2=factor,
                                                              op0=ALU.mult, op1=ALU.mult),
                         reads=[t_id] + vec_tiles, writes=[t_dg[j % 2]])
                    P.op("pe", lambda e, j=j, jj=jj, b=b: e.matmul(pbank(b, 1, jj * 128, jj * 128 + 128),
                                                                  lhsT=ones_f[:, :], rhs=dg[j % 2][:, :],
                                                                  start=True, stop=True),
                         reads=[t_id, t_dg[j % 2]], writes=[t_pb[b]])
                evac(dst[:, j4 * 512:(j4 + 1) * 512], pbank(b), [t_pb[b]], [tdst])
                bfree(b)

        def rms_stats(src_ap, src_tiles, junk_ap, junk_tiles, slot_i):
            g = t_st[slot_i]
            c = 32 + slot_i * 4
            P.op("act", lambda e: e.activation(out=junk_ap, in_=src_ap, func=AF.Square,
                                              accum_out=stt_[:, c:c + 1]),
                 reads=src_tiles, writes=junk_tiles + [g])
            P.op("act", lambda e: e.activation(out=stt_[:, c + 1:c + 2], in_=stt_[:, c:c + 1], func=AF.Sqrt,
                                              scale=1.0 / D, bias=cs_eps),
                 reads=[g, t_id], writes=[g])
            P.op("dve", lambda e: e.reciprocal(out=stt_[:, c + 2:c + 3], in_=stt_[:, c + 1:c + 2]),
                 reads=[g], writes=[g])
            return stt_[:, c + 2:c + 3], g

        def emit_M1a(r0):
            m1 = {}

            def m1_N1(ti):
                xb = xt[ti % 2]
                P.op("sp", lambda e, xb=xb, ti=ti, r0=r0: e.dma_start(
                    out=xb, in_=x_d[r0 + ti * 128:r0 + (ti + 1) * 128, :]),
                     writes=[t_xt[ti % 2]], dma="xt%d" % (ti % 2))
                rstd, g = rms_stats(xb, [t_xt[ti % 2]], xnj[ti % 2], [t_xnj[ti % 2]], ti % 2)
                dgr, tdgr = dgrs[ti % 2], t_dgr[ti % 2]
                P.op("dve", lambda e: e.tensor_scalar(out=dgr[:, :], in0=ident_f[:, :], scalar1=rstd,
                                                     scalar2=None, op0=ALU.mult),
                     reads=[t_id, g], writes=[tdgr])

            def m1_N2(ti):
                xb = xt[ti % 2]
                dgr, tdgr = dgrs[ti % 2], t_dgr[ti % 2]
                b4 = bank(4)
                m1[ti] = b4
                for c in range(16):
                    P.op("pe", lambda e, c=c: e.matmul(
                        ps[:, b4 * 512 + c * 128:b4 * 512 + (c + 1) * 128], lhsT=xb[:, c * 128:(c + 1) * 128],
                        rhs=dgr[:, :], start=True, stop=True),
                         reads=[t_xt[ti % 2], tdgr], writes=[t_pb[b4 + c // 4]])

            def m1_N3(ti):
                b4 = m1.pop(ti)
                for c in range(16):
                    src = ps[:, b4 * 512 + c * 128:b4 * 512 + (c + 1) * 128]
                    dst = hT[:, c, ti * 128:(ti + 1) * 128]
                    if c % 2 == 0:
                        P.op("act", lambda e, c=c, src=src, dst=dst: e.activation(
                            out=dst, in_=src, func=AF.Identity, scale=a12[:, c:c + 1], bias=ada_fm[:, c:c + 1]),
                             reads=[t_pb[b4 + c // 4], t_a12, t_ada], writes=[t_hT[ti]], same_ok=True)
                    else:
                        P.op("dve", lambda e, c=c, src=src, dst=dst: e.tensor_scalar(
                            out=dst, in0=src, scalar1=a12[:, c:c + 1], scalar2=ada_fm[:, c:c + 1],
                            op0=ALU.mult, op1=ALU.add),
                             reads=[t_pb[b4 + c // 4], t_a12, t_ada], writes=[t_hT[ti]], same_ok=True)
                bfree(b4, 4)

            def stage(k):
                if k == 0:
                    m1_N1(0)
                    m1_N1(1)
                else:
                    ti = k - 1
                    m1_N2(ti)
                    m1_N3(ti)
                    if ti + 2 < 5:
                        m1_N1(ti + 2)
            return stage


        for blk in range(NB):
            r0 = blk * TB
            if blk > 0:
                P.fence(regB_M, regB_E)
                P.fence(regC_M1_rest, regC_E + regC_M2)
            if blk == 0:
                st0 = emit_M1a(0)
                for k in range(6):
                    st0(k)

            hmain = lambda k: hT[:, k, 128:HT]
            for s_i in range(4):
                s = wnext("q")
                w3 = slot3(s, 16)
                for cc in range(2):
                    ch = s_i * 2 + cc
                    b = bank()
                    mm(pbank(b), [(w3[:, k, cc * 128:(cc + 1) * 128], hmain(k)) for k in range(16)],
                       [t_w[s]] + t_hT, [t_pb[b]])
                    evac(qT[:, ch, :], pbank(b), [t_pb[b]], [t_q], scale=0.125)
                    bfree(b)
                wrel()
            s = wnext("k")
            w3 = slot3(s, 16)
            for ch in range(2):
                b = bank()
                mm(pbank(b), [(w3[:, k, ch * 128:(ch + 1) * 128], hmain(k)) for k in range(16)],
                   [t_w[s]] + t_hT, [t_pb[b]])
                b_h = bank()
                mm(pbank(b_h, 1, 0, 128), [(w3[:, k, ch * 128:(ch + 1) * 128], hT[:, k, 0:128]) for k in range(16)],
                   [t_w[s]] + t_hT, [t_pb[b_h]])
                for hf_ in range(2):
                    kv = ch * 2 + hf_
                    o = hf_ * 64
                    evac(kT2[o:o + 64, kv, 128:HT], ps[o:o + 64, b * 512:b * 512 + 512], [t_pb[b]], [t_kn])
                    evac(kT2[o:o + 64, kv, 0:128], ps[o:o + 64, b_h * 512:b_h * 512 + 128], [t_pb[b_h]], [t_kn])
                bfree(b)
                bfree(b_h)
            wrel()
            for kv in range(4):
                o = (kv % 2) * 64
                P.op("sp", lambda e, kv=kv, o=o: e.dma_start(out=kT2[64 - o:128 - o, kv, :], in_=kT2[o:o + 64, kv, :]),
                     reads=[t_kn], writes=[t_kd], dma="kdup")
            s = wnext("v")
            w3 = slot3(s, 16)
            for ti in range(5):
                b = bank()
                mm(pbank(b, 1, 0, 256), [(hT[:, k, ti * 128:(ti + 1) * 128], w3[:, k, :]) for k in range(16)],
                   [t_w[s]] + t_hT, [t_pb[b]])
                src = pbank(b, 1, 0, 256).rearrange("p (g d) -> p g d", g=4)
                evac(Vt[:, ti, :, 0:64], src, [t_pb[b]], [t_v])
                evac(Vt[:, ti, :, 64:128], src, [t_pb[b]], [t_v])
                bfree(b)
            wrel()

            att_state = {}

            def att_A(i):
                qt, kv = divmod(i, 4)
                mcol = 256 if (blk == 0 and qt == 0) else 0
                b2 = bank(2)
                for j in range(4):
                    h = kv * 4 + j
                    off = (h % 2) * 64
                    o_ap = ps[:, b2 * 512 + j * 256:b2 * 512 + (j + 1) * 256]
                    P.op("pe", lambda e, o_ap=o_ap, off=off, h=h, qt=qt, kv=kv: e.matmul(
                        o_ap, lhsT=qT[off:off + 64, h // 2, qt * 128:(qt + 1) * 128],
                        rhs=kT2[off:off + 64, kv, qt * 128:(qt + 2) * 128], start=True, stop=False),
                         reads=[t_q, t_kn, t_kd], writes=[t_pb[b2 + j // 2]])
                    P.op("pe", lambda e, o_ap=o_ap, mcol=mcol: e.matmul(
                        o_ap, lhsT=ident_b[:, :], rhs=amask_b[:, mcol:mcol + 256], start=False, stop=True),
                         reads=[t_id], writes=[t_pb[b2 + j // 2]])
                att_state[i] = b2

            def att_ctx(i):
                qt, kv = divmod(i, 4)
                return dict(qt=qt, kv=kv, S=stA[i % 2], tS=t_sa[i % 2], Pn=Pb[i % 2], tP=t_P[i % 2],
                            PTb=PTs[i % 2], tPT=t_PT[i % 2], sink4=cs(C_SINK + kv * 4, 4))

            def att_B1(i):
                c = att_ctx(i)
                S, tS, sink4 = c["S"], c["tS"], c["sink4"]
                b2 = att_state[i]
                pbs = [t_pb[b2], t_pb[b2 + 1]]
                P.op("dve", lambda e: e.tensor_reduce(out=S[:, 0:4], in_=pbank(b2, 2).rearrange(K3, k=4), axis=AX.X,
                                                     op=ALU.max), reads=pbs, writes=[tS])
                P.op("dve", lambda e: e.tensor_tensor(out=S[:, 0:4], in0=S[:, 0:4], in1=sink4, op=ALU.max),
                     reads=[tS, t_cst], writes=[tS])
                P.op("dve", lambda e: e.tensor_scalar(out=S[:, 4:8], in0=S[:, 0:4], scalar1=-1.0, scalar2=None,
                                                     op0=ALU.mult), reads=[tS], writes=[tS])
                P.op("dve", lambda e: e.tensor_tensor(out=S[:, 8:12], in0=sink4, in1=S[:, 0:4], op=ALU.subtract),
                     reads=[tS, t_cst], writes=[tS], same_ok=True)

            def att_B2(i):
                c = att_ctx(i)
                S, tS, Pn, tP = c["S"], c["tS"], c["Pn"], c["tP"]
                b2 = att_state.pop(i)
                pbs = [t_pb[b2], t_pb[b2 + 1]]
                for j in range(4):
                    P.op("act", lambda e, j=j: e.activation(
                        out=Pn[:, j, :], in_=ps[:, b2 * 512 + j * 256:b2 * 512 + (j + 1) * 256], func=AF.Exp,
                        bias=S[:, 4 + j:5 + j], scale=1.0, accum_out=S[:, 12 + j:13 + j]),
                         reads=pbs + [tS], writes=[tP, tS], same_ok=True)
                P.op("act", lambda e: e.activation(out=S[:, 16:20], in_=S[:, 8:12], func=AF.Exp),
                     reads=[tS], writes=[tS], same_ok=True)
                bfree(b2, 2)

            def att_B3(i):
                c = att_ctx(i)
                S, tS, Pn, tP = c["S"], c["tS"], c["Pn"], c["tP"]
                P.op("dve", lambda e: e.tensor_tensor(out=S[:, 20:24], in0=S[:, 12:16], in1=S[:, 16:20], op=ALU.add),
                     reads=[tS], writes=[tS])
                P.op("dve", lambda e: e.reciprocal(out=S[:, 24:28], in_=S[:, 20:24]), reads=[tS], writes=[tS])
                P.op("dve", lambda e: e.tensor_tensor(
                    out=Pn[:, :, :], in0=Pn[:, :, :], in1=S[:, 24:28].unsqueeze(2).broadcast_to([128, 4, 256]),
                    op=ALU.mult), reads=[tP, tS], writes=[tP])

            def att_B4(i):
                c = att_ctx(i)
                Pn, tP, PTb, tPT = c["Pn"], c["tP"], c["PTb"], c["tPT"]
                bt = bank()
                ptv = pbank(bt).bitcast(BF16)
                for half in range(2):
                    for j in range(4):
                        o = (half * 4 + j) * 128
                        P.op("pe", lambda e, o=o, j=j, half=half: e.transpose(
                            out=ptv[:, o:o + 128], in_=Pn[:, j, half * 128:(half + 1) * 128],
                            identity=ident_b[:, :]), reads=[tP, t_id], writes=[t_pb[bt]])
                evac(PTb, ptv, [t_pb[bt]], [tPT])
                bfree(bt)

            def att_B5(i):
                c = att_ctx(i)
                qt, kv, PTb, tPT = c["qt"], c["kv"], c["PTb"], c["tPT"]
                bo = bank()
                mm(pbank(bo), [(Vt[:, qt, kv, :], PTb[:, 0:512]), (Vt[:, qt + 1, kv, :], PTb[:, 512:1024])],
                   [t_v, tPT], [t_pb[bo]])
                for j in range(4):
                    h = kv * 4 + j
                    o2 = (h % 2) * 64
                    srcp = ps[o2:o2 + 64, bo * 512 + j * 128:bo * 512 + (j + 1) * 128]
                    dstp = attnT[o2:o2 + 64, h // 2, qt * 128:(qt + 1) * 128]
                    if j % 2 == 0:
                        P.op("act", lambda e, srcp=srcp, dstp=dstp: e.activation(out=dstp, in_=srcp, func=AF.Copy),
                             reads=[t_pb[bo]], writes=[t_attn[qt]], same_ok=True)
                    else:
                        P.op("dve", lambda e, srcp=srcp, dstp=dstp: e.tensor_copy(out=dstp, in_=srcp),
                             reads=[t_pb[bo]], writes=[t_attn[qt]], same_ok=True)
                bfree(bo)

            def u_chunk(s, n_in, ch):
                w3 = slot3(s, 16)
                g = ch // 2
                wdw = 2 << g
                u = ub[ch % 2]
                tu = t_ub[ch % 2]
                b = bank()
                mm(pbank(b), [(w3[:, k, n_in * 128:(n_in + 1) * 128], hmain(k)) for k in range(16)],
                   [t_w[s]] + t_hT, [t_pb[b]])
                P.op("act", lambda e: e.activation(out=u[:, 16:528], in_=pbank(b), func=AF.Copy),
                     reads=[t_pb[b]], writes=[tu])
                bfree(b)
                b2 = bank()
                mm(pbank(b2, 1, 0, 16), [(w3[:, k, n_in * 128:(n_in + 1) * 128], hT[:, k, 112:128]) for k in range(16)],
                   [t_w[s]] + t_hT, [t_pb[b2]])
                if blk == 0:
                    P.op("dve", lambda e: e.tensor_scalar(out=u[:, 0:16], in0=pbank(b2, 1, 0, 16),
                                                         scalar1=cs(C_UF, 1), scalar2=None, op0=ALU.mult),
                         reads=[t_pb[b2], t_cst], writes=[tu], same_ok=True)
                    bfree(b2)
                else:
                    P.op("dve", lambda e: e.tensor_copy(out=u[:, 0:16], in_=pbank(b2, 1, 0, 16)),
                         reads=[t_pb[b2]], writes=[tu], same_ok=True)
                bfree(b2)
                cur, tcur = u, tu
                step = 1
                i = 0
                while step < wdw:
                    nxt, tn = sAB[i % 2], t_s[i % 2]
                    P.op("pool", lambda e, cur=cur, nxt=nxt, step=step: e.tensor_tensor(
                        out=nxt[:, step:528], in0=cur[:, step:528], in1=cur[:, 0:528 - step], op=ALU.add),
                         reads=[tcur], writes=[tn])
                    cur, tcur = nxt, tn
                    step *= 2
                    i += 1
                P.op("dve", lambda e, cur=cur: e.scalar_tensor_tensor(
                    out=pooled[:, ch, :], in0=cur[:, 16:528], scalar=1.0 / wdw, in1=u[:, 16:528],
                    op0=ALU.mult, op1=ALU.subtract),
                     reads=[tcur, tu], writes=[t_pooled[ch]])
                if blk == 0:
                    oth, toth = sAB[i % 2], t_s[i % 2]
                    P.op("dve", lambda e, cur=cur, oth=oth: e.tensor_tensor(
                        out=oth[:, 0:16], in0=cur[:, 16:32], in1=cs(C_INVC + g * 16, 16), op=ALU.mult),
                         reads=[tcur, t_cst], writes=[toth])
                    P.op("dve", lambda e, oth=oth: e.tensor_tensor(
                        out=pooled[:, ch, 0:16], in0=oth[:, 0:16], in1=u[:, 16:32], op=ALU.subtract),
                         reads=[toth, tu], writes=[t_pooled[ch]])

            u_slot = None
            for ch in range(8):
                if ch % 2 == 0:
                    u_slot = wnext("u")
                u_chunk(u_slot, ch % 2, ch)
                if ch % 2 == 1:
                    wrel()
            P.fence(t_siga + t_sigp[0:2], t_xnj + t_xt)
            P.fence(t_sigp[2:4], t_ub + t_s)

            def gate_unit(gi):
                which, hh = divmod(gi, 8)
                s = wnext("g")
                w3 = slot3(s, 16)
                for nn in range(2):
                    ch = hh * 2 + nn
                    b = bank()
                    mm(pbank(b), [(w3[:, k, nn * 128:(nn + 1) * 128], hmain(k)) for k in range(16)],
                       [t_w[s]] + t_hT, [t_pb[b]])
                    if which == 0:
                        dst, tdst = siga[:, ch, :], t_siga[ch // 4]
                    elif ch < 8:
                        dst, tdst = sigp_lo[:, ch, :], t_sigp[ch // 4]
                    else:
                        dst, tdst = sigp_hi[:, ch - 8, :], t_sigp[ch // 4]
                    P.op("act", lambda e, b=b, dst=dst: e.activation(out=dst, in_=pbank(b), func=AF.Tanh, scale=0.5),
                         reads=[t_pb[b]], writes=[tdst], same_ok=True)
                    bfree(b)
                wrel()
                ada_consume()

            for sst in range(16 + 3):
                if sst < 16:
                    att_A(sst)
                    gate_unit(sst)
                if sst == 10:
                    if blk == 0:
                        ada_finish_a()
                    regen_bc(ada_fm[:, 32:48], [t_ada2a], factor=0.5)
                if 0 <= sst - 2 < 16:
                    att_B3(sst - 2)
                if 0 <= sst - 1 < 16:
                    att_B1(sst - 1)
                    att_B2(sst - 1)
                if 0 <= sst - 2 < 16:
                    att_B4(sst - 2)
                if 0 <= sst - 3 < 16:
                    att_B5(sst - 3)

            s = wnext("wp")
            wp4 = wring[s][:, 0:2048].rearrange("p (g k d) -> p g k d", g=4, k=2)
            for g in range(4):
                for oc in range(2):
                    b = bank()
                    mm(pbank(b), [(wp4[:, g, kc, oc * 128:(oc + 1) * 128], pooled[:, g * 2 + kc, :]) for kc in range(2)],
                       [t_w[s]] + t_pooled, [t_pb[b]])
                    ch = g * 2 + oc
                    P.op("act", lambda e, b=b, ch=ch: e.activation(out=pmixT[:, ch, :], in_=pbank(b), func=AF.Identity,
                                                                  scale=cs(C_PSC + ch, 1), bias=0.0),
                         reads=[t_pb[b], t_cst], writes=[t_pmix], same_ok=True)
                    bfree(b)
            wrel()
            ada_consume()

            P.fence(t_merged, [t_q, t_kn, t_kd, t_v])
            P.fence(t_tA + t_tB, t_P + t_PT)
            for og in range(4):
                sA_ = wnext("brA")
                sB_ = wnext("brB")
                wa3, wb3 = slot3(sA_, 8), slot3(sB_, 8)
                for n in range(4):
                    ba = bank()
                    mm(pbank(ba), [(wa3[:, kc, n * 128:(n + 1) * 128], attnT[:, kc, :]) for kc in range(8)],
                       [t_w[sA_]] + t_attn, [t_pb[ba]])
                    bb = bank()
                    mm(pbank(bb), [(wb3[:, kc, n * 128:(n + 1) * 128], pmixT[:, kc, :]) for kc in range(8)],
                       [t_w[sB_], t_pmix], [t_pb[bb]])
                    i2 = n % 2
                    ch = og * 4 + n
                    ga_ap = siga[:, ch, :]
                    gp_ap = sigp_lo[:, ch, :] if ch < 8 else sigp_hi[:, ch - 8, :]
                    P.op("dve", lambda e, ba=ba, ga_ap=ga_ap, i2=i2: e.scalar_tensor_tensor(
                        out=tA[i2], in0=ga_ap, scalar=1.0, in1=pbank(ba), op0=ALU.add, op1=ALU.mult),
                         reads=[t_pb[ba], t_siga[og]], writes=[t_tA[i2]])
                    P.op("dve", lambda e, bb=bb, gp_ap=gp_ap, i2=i2: e.scalar_tensor_tensor(
                        out=tB[i2], in0=gp_ap, scalar=1.0, in1=pbank(bb), op0=ALU.add, op1=ALU.mult),
                         reads=[t_pb[bb], t_sigp[og]], writes=[t_tB[i2]])
                    bfree(ba)
                    bfree(bb)
                    P.op("pool", lambda e, og=og, n=n, i2=i2: e.tensor_tensor(out=mergedT[:, og * 4 + n, :], in0=tA[i2],
                                                                             in1=tB[i2], op=ALU.add),
                         reads=[t_tA[i2], t_tB[i2]], writes=[t_merged[og]], same_ok=True)
                wrel(2)
                ada_consume(2)

            for ti in range(4):
                P.op("sp", lambda e, ti=ti, r0=r0: e.dma_start(
                    out=xacc[:, ti, :], in_=x_d[r0 + 128 + ti * 128:r0 + 128 + (ti + 1) * 128, :]),
                     writes=[t_xa[ti]], dma="xa%d" % ti)
            k_ev = 0
            for db in range(4):
                bks = [bank() for _ in range(4)]
                for kh in range(2):
                    s = wnext("wo")
                    w3 = slot3(s, 8)
                    for ti in range(4):
                        mm(pbank(bks[ti]), [(mergedT[:, kh * 8 + k, ti * 128:(ti + 1) * 128], w3[:, k, :])
                                            for k in range(8)],
                           [t_w[s]] + t_merged, [t_pb[bks[ti]]], start=(kh == 0), stop=(kh == 1))
                    wrel()
                    ada_consume()
                for ti in range(4):
                    b = bks[ti]
                    i4 = k_ev % 2
                    k_ev += 1
                    P.op("dve", lambda e, b=b, db=db, i4=i4: e.tensor_tensor(out=tA[i4], in0=pbank(b),
                                                                            in1=bc[:, db * 512:(db + 1) * 512],
                                                                            op=ALU.mult),
                         reads=[t_pb[b], t_bc], writes=[t_tA[i4]])
                    bfree(b)
                    P.op("pool", lambda e, ti=ti, db=db, i4=i4: e.tensor_tensor(
                        out=xacc[:, ti, db * 512:(db + 1) * 512], in0=xacc[:, ti, db * 512:(db + 1) * 512],
                        in1=tA[i4], op=ALU.add),
                         reads=[t_tA[i4]], writes=[t_xa[ti]], same_ok=True)

            if blk == 0:
                ada_finish()
            P.fence(regB_E, regB_M)
            P.fence(regC_E, regC_M2)
            n2 = {}

            def n2_N1(ti):
                rstd, g = rms_stats(xacc[:, ti, :], [t_xa[ti]], xn2j[ti % 2], [t_xn2j[ti % 2]], 4 + ti % 2)
                dgr, tdgr = dgrs[ti % 2], t_dgr[ti % 2]
                P.op("dve", lambda e: e.tensor_scalar(out=dgr[:, :], in0=ident_f[:, :], scalar1=rstd,
                                                     scalar2=None, op0=ALU.mult),
                     reads=[t_id, g], writes=[tdgr])

            def n2_N2(ti):
                dgr, tdgr = dgrs[ti % 2], t_dgr[ti % 2]
                b4 = bank(4)
                n2[ti] = b4
                for c in range(16):
                    P.op("pe", lambda e, c=c: e.matmul(
                        ps[:, b4 * 512 + c * 128:b4 * 512 + (c + 1) * 128],
                        lhsT=xacc[:, ti, c * 128:(c + 1) * 128], rhs=dgr[:, :], start=True, stop=True),
                         reads=[t_xa[ti], tdgr], writes=[t_pb[b4 + c // 4]])

            def n2_N3(ti):
                b4 = n2.pop(ti)
                hf, thf = h2Tf[ti % 2], t_h2Tf[ti % 2]
                for c in range(16):
                    src = ps[:, b4 * 512 + c * 128:b4 * 512 + (c + 1) * 128]
                    if c % 2 == 0:
                        P.op("act", lambda e, c=c, src=src: e.activation(
                            out=hf[:, c, :], in_=src, func=AF.Identity, scale=a12[:, 16 + c:17 + c],
                            bias=ada_fm[:, 48 + c:49 + c]),
                             reads=[t_pb[b4 + c // 4], t_a2, t_ada2], writes=[thf], same_ok=True)
                    else:
                        P.op("dve", lambda e, c=c, src=src: e.tensor_scalar(
                            out=hf[:, c, :], in0=src, scalar1=a12[:, 16 + c:17 + c],
                            scalar2=ada_fm[:, 48 + c:49 + c], op0=ALU.mult, op1=ALU.add),
                             reads=[t_pb[b4 + c // 4], t_a2, t_ada2], writes=[thf], same_ok=True)
                bfree(b4, 4)
                P.op("pool", lambda e: e.tensor_copy(out=h2T[:, :, ti * 128:(ti + 1) * 128], in_=hf),
                     reads=[thf], writes=[t_h2T[ti]])
                br = bank()
                wr3 = cs(C_WR, 320).rearrange(K3, k=16)
                mm(pbank(br, 1, 0, 20), [(hf[:, k, :], wr3[:, k, :]) for k in range(16)], [thf, t_cst], [t_pb[br]])
                n2[("br", ti)] = br

            def router_ops(ti):
                br = n2.pop(("br", ti))
                rtb, trt = rts[ti % 2], t_rts[ti % 2]
                R = lambda a_, b_: rtb[:, a_:b_]
                ops = []
                ro = lambda fn, rd=(): ops.append(lambda: P.op("dve", fn, reads=[trt] + list(rd), writes=[trt]))
                ra = lambda fn: ops.append(lambda: P.op("act", fn, reads=[trt], writes=[trt]))

                def first():
                    P.op("dve", lambda e: e.tensor_tensor(out=R(0, 20), in0=pbank(br, 1, 0, 20), in1=cs(C_BR, 20),
                                                         op=ALU.add), reads=[trt, t_pb[br], t_cst], writes=[trt])
                    bfree(br)
                ops.append(first)
                ro(lambda e: e.tensor_reduce(out=R(20, 21), in_=R(0, 4), axis=AX.X, op=ALU.max))
                ro(lambda e: e.tensor_scalar(out=R(21, 22), in0=R(20, 21), scalar1=-1.0, scalar2=None, op0=ALU.mult))
                ra(lambda e: e.activation(out=R(24, 28), in_=R(0, 4), func=AF.Exp, bias=R(21, 22), scale=1.0,
                                          accum_out=R(22, 23)))
                ro(lambda e: e.reciprocal(out=R(23, 24), in_=R(22, 23)))
                ro(lambda e: e.tensor_scalar(out=R(28, 32), in0=R(0, 4), scalar1=R(20, 21), scalar2=None,
                                             op0=ALU.is_equal))
                ro(lambda e: e.tensor_tensor(out=R(32, 48).rearrange("p (g e) -> p g e", g=4),
                                             in0=R(4, 20).rearrange("p (g e) -> p g e", g=4),
                                             in1=R(28, 32).unsqueeze(2).broadcast_to([128, 4, 4]), op=ALU.mult))
                ro(lambda e: e.tensor_reduce(out=R(48, 52), in_=R(32, 48).rearrange("p (g e) -> p e g", g=4),
                                             axis=AX.X, op=ALU.add))
                ro(lambda e: e.tensor_reduce(out=R(52, 53), in_=R(48, 52), axis=AX.X, op=ALU.max))
                ro(lambda e: e.tensor_scalar(out=R(56, 60), in0=R(48, 52), scalar1=R(52, 53), scalar2=None,
                                             op0=ALU.is_equal))
                ro(lambda e: e.scalar_tensor_tensor(out=R(60, 64), in0=R(56, 60), scalar=NEG, in1=R(48, 52),
                                                    op0=ALU.mult, op1=ALU.add))
                ro(lambda e: e.tensor_reduce(out=R(53, 54), in_=R(60, 64), axis=AX.X, op=ALU.max))
                ro(lambda e: e.tensor_scalar(out=R(64, 68), in0=R(60, 64), scalar1=R(53, 54), scalar2=None,
                                             op0=ALU.is_equal))
                ro(lambda e: e.tensor_tensor(out=R(54, 55), in0=R(53, 54), in1=R(52, 53), op=ALU.subtract))
                ra(lambda e: e.activation(out=R(55, 56), in_=R(54, 55), func=AF.Exp))
                ro(lambda e: e.tensor_scalar(out=R(68, 69), in0=R(55, 56), scalar1=1.0, scalar2=None, op0=ALU.add))
                ro(lambda e: e.reciprocal(out=R(69, 70), in_=R(68, 69)))
                ro(lambda e: e.tensor_tensor(out=R(70, 71), in0=R(55, 56), in1=R(69, 70), op=ALU.mult))
                ro(lambda e: e.tensor_tensor(out=R(71, 72), in0=R(69, 70), in1=R(23, 24), op=ALU.mult))
                ro(lambda e: e.tensor_tensor(out=R(72, 73), in0=R(70, 71), in1=R(23, 24), op=ALU.mult))
                ro(lambda e: e.tensor_scalar(out=R(76, 80), in0=R(56, 60), scalar1=R(71, 72), scalar2=None,
                                             op0=ALU.mult))
                ro(lambda e: e.scalar_tensor_tensor(out=R(80, 84), in0=R(64, 68), scalar=R(72, 73), in1=R(76, 80),
                                                    op0=ALU.mult, op1=ALU.add))
                ops.append(lambda: P.op("dve", lambda e: e.tensor_tensor(
                    out=comb[:, ti, :].rearrange("p (g e) -> p g e", g=4),
                    in0=R(28, 32).unsqueeze(2).broadcast_to([128, 4, 4]),
                    in1=R(80, 84).unsqueeze(1).broadcast_to([128, 4, 4]), op=ALU.mult),
                    reads=[trt], writes=[t_comb], same_ok=True))
                return ops

            def router_pair(t0_, t1_):
                la, lb = router_ops(t0_), router_ops(t1_)
                for fa, fb in zip(la, lb):
                    fa()
                    fb()

            n2_N1(0)
            n2_N1(1)
            for ti in range(4):
                n2_N2(ti)
                n2_N3(ti)
                if ti + 2 < 4:
                    n2_N1(ti + 2)
                if ti % 2 == 1:
                    router_pair(ti - 1, ti)

            regen_bc(ada_fm[:, 80:96], [t_ada2])
            kst = 0
            for ex in range(NEXP):
                if ex == HOIST_AT:
                    P.op("sp", lambda e: e.dma_start(out=bc2, in_=fg_d), writes=[t_bc2], dma="bc2")
                    if blk + 1 < NB:
                        P.fence(t_xnj + t_xt, t_xn2j + t_h2Tf)
                        hoist_stage = emit_M1a((blk + 1) * TB)
                if blk + 1 < NB and HOIST_AT <= ex < HOIST_AT + 6:
                    hoist_stage(ex - HOIST_AT)
                ab, tab = actb[ex % 2], t_act[ex % 2]
                for hh in range(2):
                    sg_ = wnext("eg")
                    su_ = wnext("eu")
                    wg3, wu3 = slot3(sg_, 16), slot3(su_, 16)
                    for ff in range(2):
                        fc = hh * 2 + ff
                        bg = bank()
                        mm(pbank(bg), [(wg3[:, k, ff * 128:(ff + 1) * 128], h2T[:, k, :]) for k in range(16)],
                           [t_w[sg_]] + t_h2T, [t_pb[bg]])
                        bu = bank()
                        mm(pbank(bu), [(wu3[:, k, ff * 128:(ff + 1) * 128], h2T[:, k, :]) for k in range(16)],
                           [t_w[su_]] + t_h2T, [t_pb[bu]])
                        i2 = fc % 2
                        P.op("act", lambda e, bg=bg, i2=i2: e.activation(out=sl[i2], in_=pbank(bg), func=AF.Silu),
                             reads=[t_pb[bg]], writes=[t_sl[i2]])
                        P.op("dve", lambda e, bu=bu, i2=i2, ab=ab, fc=fc: e.tensor_tensor(out=ab[:, fc, :], in0=sl[i2],
                                                                                         in1=pbank(bu), op=ALU.mult),
                             reads=[t_sl[i2], t_pb[bu]], writes=[tab], same_ok=True)
                        bfree(bg)
                        bfree(bu)
                    wrel(2)
                for dh in range(2):
                    sd_ = wnext("ed")
                    wd3 = slot3(sd_, 4)
                    for ti in range(4):
                        for dd in range(2):
                            db = dh * 2 + dd
                            b = bank()
                            mm(pbank(b), [(ab[:, fc, ti * 128:(ti + 1) * 128], wd3[:, fc, dd * 512:(dd + 1) * 512])
                                          for fc in range(4)], [t_w[sd_], tab], [t_pb[b]])
                            i4 = kst % 4
                            kst += 1
                            P.op("dve", lambda e, b=b, ti=ti, ex=ex, db=db, i4=i4: e.scalar_tensor_tensor(
                                out=stmp[i4], in0=pbank(b), scalar=comb[:, ti, ex:ex + 1],
                                in1=bc[:, db * 512:(db + 1) * 512], op0=ALU.mult, op1=ALU.mult),
                                 reads=[t_pb[b], t_comb, t_bc], writes=[t_stmp[i4]])
                            bfree(b)
                            P.op("pool", lambda e, ti=ti, db=db, i4=i4: e.tensor_tensor(
                                out=xacc[:, ti, db * 512:(db + 1) * 512], in0=xacc[:, ti, db * 512:(db + 1) * 512],
                                in1=stmp[i4], op=ALU.add),
                                 reads=[t_stmp[i4]], writes=[t_xa[ti]], same_ok=True)
                    wrel()

            for ti in range(4):
                rstd, g = rms_stats(xacc[:, ti, :], [t_xa[ti]], fjunk, [t_fj], 6 + ti % 2)
                P.op("dve", lambda e, ti=ti, rstd=rstd: e.scalar_tensor_tensor(
                    out=xacc[:, ti, :], in0=xacc[:, ti, :], scalar=rstd, in1=bc2, op0=ALU.mult, op1=ALU.mult),
                     reads=[g, t_bc2], writes=[t_xa[ti]])
                P.op("sp", lambda e, ti=ti, r0=r0: e.dma_start(out=y_d[r0 + ti * 128:r0 + (ti + 1) * 128, :],
                                                               in_=xacc[:, ti, :]),
                     reads=[t_xa[ti]], dma="out%d" % ti)

        assert wq["acq"] == len(specs) and wq["released"] == len(specs), (wq, len(specs))
        P.emit(nc, sems, dsem, ["out%d" % i for i in range(4)])
    return nc


_NC_CACHE = {}


def _host_consts(core, c, b_ada, norm1_g, sinks, pool_scale, norm2_g, w_router_group, b_router_group,
                 w_router_expert, b_router_expert, final_g):
    b, q = core // 4, core % 4
    fm = lambda v: np.ascontiguousarray(v.reshape(-1, 128).T)
    cst = np.zeros((128, C_TOT), np.float32)
    cst[:, C_C:C_C + 16] = fm(c[b])
    cst[:, C_BADA:C_BADA + 96] = fm(b_ada[0])
    cst[:, C_G1:C_G1 + 16] = fm(norm1_g[0])
    cst[:, C_SINK:C_SINK + 16] = sinks[0][None, :]
    cst[:, C_PSC:C_PSC + 8] = fm(pool_scale[0])
    cst[:, C_G2:C_G2 + 16] = fm(norm2_g[0])
    cst[:, C_BR:C_BR + 4] = b_router_group[0][None, :]
    cst[:, C_BR + 4:C_BR + 20] = b_router_expert[0][None, :]
    cst[:, C_FG:C_FG + 16] = fm(final_g)
    cst[:, C_UF] = 0.0 if q == 0 else 1.0
    for g, w in enumerate((2, 4, 8, 16)):
        t = np.arange(16)
        cst[:, C_INVC + g * 16:C_INVC + (g + 1) * 16] = (1.0 / np.minimum(t + 1, w) if q == 0
                                                         else np.full(16, 1.0 / w))[None, :]
    r = np.arange(128)[:, None]
    j = np.arange(256)[None, :]
    valid = (j > r) & (j <= r + 128)
    am = np.where(valid, 0.0, NEG).astype(np.float32)
    cst[:, C_AM:C_AM + 256] = am
    am0 = am.copy()
    if q == 0:
        am0[:, :128] = NEG
    cst[:, C_AM0:C_AM0 + 256] = am0
    wr = np.concatenate([w_router_group[0], w_router_expert[0]], axis=1)
    cst[:, C_WR:C_WR + 320] = wr.reshape(16, 128, 20).transpose(1, 0, 2).reshape(128, 320)
    return cst


def kernel(x, c, w_ada, b_ada, norm1_g, w_in, sinks, w_pool, pool_scale, w_attn_branch, w_pool_branch, w_out,
           norm2_g, w_router_group, b_router_group, w_router_expert, b_router_expert, w_e_gate, w_e_up,
           w_e_down, final_g):
    f = lambda a: np.ascontiguousarray(np.asarray(a, dtype=np.float32))
    x = f(x)
    if "nc" not in _NC_CACHE:
        _NC_CACHE["nc"] = build_nc()
    nc = _NC_CACHE["nc"]
    shared = {
        "w_ada": f(w_ada)[0], "w_in": f(w_in)[0], "w_pool": f(w_pool)[0], "w_ab": f(w_attn_branch)[0],
        "w_pb": f(w_pool_branch)[0], "w_out": f(w_out)[0], "w_eg": f(w_e_gate)[0], "w_eu": f(w_e_up)[0],
        "w_ed": f(w_e_down)[0],
    }
    small = [np.asarray(a, dtype=np.float32) for a in (c, b_ada, norm1_g, sinks, pool_scale, norm2_g,
                                                        w_router_group, b_router_group, w_router_expert,
                                                        b_router_expert, final_g)]
    fg_bc = np.ascontiguousarray(np.broadcast_to(np.asarray(final_g, dtype=np.float32)[None, :], (128, D)))
    in_maps = []
    for core in range(NCORE):
        b, q = core // 4, core % 4
        xc = np.zeros((NTOK + 128, D), np.float32)
        if q > 0:
            xc[:128] = x[b, q * NTOK - 128:q * NTOK]
        xc[128:] = x[b, q * NTOK:(q + 1) * NTOK]
        m = dict(shared)
        m["x"] = xc
        m["cst"] = _host_consts(core, *small)
        m["fg_bc"] = fg_bc
        in_maps.append(m)
    res = run_bass_kernel_spmd(nc, in_maps, core_ids=list(range(NCORE)))
    out = np.empty((2, 4 * NTOK, D), np.float32)
    for core in range(NCORE):
        b, q = core // 4, core % 4
        out[b, q * NTOK:(q + 1) * NTOK] = res.results[core]["y"]
    return out
```

```python
import numpy as np
from contextlib import ExitStack
import concourse.bass as bass
import concourse.mybir as mybir
from concourse.bass_utils import run_bass_kernel_spmd

F32 = mybir.dt.float32
BF16 = mybir.dt.bfloat16
U8 = mybir.dt.uint8
AF = mybir.ActivationFunctionType
ALU = mybir.AluOpType
AX = mybir.AxisListType

D = 2048
NCORE = 8
NTOK = 2048
TB = 512
NB = NTOK // TB
HT = TB + 128
NEXP = 16
EPS = 1e-6
NEG = -1e30

C_C, C_BADA, C_G1, C_SINK, C_PSC, C_G2, C_BR, C_FG, C_UF, C_INVC, C_AM, C_AM0, C_WR = (
    0, 16, 112, 128, 144, 152, 168, 188, 204, 205, 269, 525, 781)
C_TOT = 781 + 320


class T:
    __slots__ = ("w", "r")

    def __init__(self):
        self.w = None
        self.r = {}


class Prog:
    ENGS = ("pe", "act", "dve", "pool", "sp")

    def __init__(self):
        self.ops = {e: [] for e in self.ENGS}
        self.seen = {e: {} for e in self.ENGS}
        self.dma_cnt = {}
        self.need = {e: set() for e in self.ENGS}

    def op(self, eng, fn, reads=(), writes=(), dma=None, same_ok=False):
        deps = {}

        def add(k, v):
            if deps.get(k, -1) < v:
                deps[k] = v

        for t in reads:
            if t.w is not None:
                add(t.w[:2], t.w[2])
        for t in writes:
            if t.w is not None:
                add(t.w[:2], t.w[2])
            for k, v in t.r.items():
                add(k, v)
        idx = len(self.ops[eng])
        waits = []
        sn = self.seen[eng]
        for k, val in deps.items():
            if k[0] == "e" and k[1] == eng and (eng == "pe" or same_ok):
                continue
            if sn.get(k, -1) >= val:
                continue
            sn[k] = val
            waits.append((k[0], k[1], val))
            if k[0] == "e":
                self.need[k[1]].add(val)
        if dma is not None:
            self.dma_cnt[dma] = self.dma_cnt.get(dma, 0) + 16
            me = ("d", dma, self.dma_cnt[dma])
        else:
            me = ("e", eng, idx)
        self.ops[eng].append((waits, fn, dma))
        for t in reads:
            if t.r.get(me[:2], -1) < me[2]:
                t.r[me[:2]] = me[2]
        for t in writes:
            t.w = me
            t.r = {}
        return me

    def fence(self, new, old):
        deps = {}
        for t in old:
            if t.w is not None and deps.get(t.w[:2], -1) < t.w[2]:
                deps[t.w[:2]] = t.w[2]
            for k, v in t.r.items():
                if deps.get(k, -1) < v:
                    deps[k] = v
        for t in new:
            t.w = None
            t.r = dict(deps)

    def emit(self, nc, sems, dma_sems, final_waits):
        rank = {}
        for e in self.ENGS:
            s = sorted(self.need[e])
            rank[e] = {v: i + 1 for i, v in enumerate(s)}
        handles = {"pe": "tensor", "act": "scalar", "dve": "vector", "pool": "gpsimd", "sp": "sync"}

        def replay(engname, e):
            rk = rank[engname]
            for idx, (waits, fn, dma) in enumerate(self.ops[engname]):
                for kind, key, val in waits:
                    if kind == "e":
                        e.wait_ge(sems[key], rank[key][val])
                    else:
                        e.wait_ge(dma_sems[key], val)
                ins = fn(e)
                if dma is not None:
                    ins.then_inc(dma_sems[dma], 16)
                elif idx in rk:
                    ins.then_inc(sems[engname], 1)
            if engname == "sp":
                for key in final_waits:
                    e.wait_ge(dma_sems[key], self.dma_cnt[key])

        with nc.Block() as block:
            for engname in self.ENGS:
                getattr(block, handles[engname])(lambda e, n=engname: replay(n, e))


ADA_PRO = 16
HOIST_AT = 6


def build_nc():
    nc = bass.Bass("TRN2", target_bir_lowering=False)

    def din(name, shape):
        return nc.dram_tensor(name, shape, F32, kind="ExternalInput").ap()

    x_d = din("x", [NTOK + 128, D])
    cst_d = din("cst", [128, C_TOT])
    w_ada = din("w_ada", [D, 6 * D])
    w_in = din("w_in", [D, 6656])
    w_pool = din("w_pool", [4, 256, 256])
    w_ab = din("w_ab", [1024, D])
    w_pb = din("w_pb", [1024, D])
    w_out = din("w_out", [D, D])
    w_eg = din("w_eg", [NEXP, D, 512])
    w_eu = din("w_eu", [NEXP, D, 512])
    w_ed = din("w_ed", [NEXP, 512, D])
    fg_d = din("fg_bc", [128, D])
    y_d = nc.dram_tensor("y", [NTOK, D], F32, kind="ExternalOutput").ap()

    P = Prog()
    with ExitStack() as st:
        def sb(name, shape, dt):
            return st.enter_context(nc.sbuf_tensor(name, shape, dt))

        NSLOT = 6
        SLOTE = 4096
        wring = [sb("wring%d" % i, [128, SLOTE], BF16) for i in range(NSLOT)]
        xacc = sb("xacc", [128, 4, D], F32)
        bc = sb("bc", [128, D], F32)
        cst = sb("cst_sb", [128, C_TOT], F32)
        ident_f = sb("ident_f", [128, 128], F32)
        ones_f = sb("ones_f", [128, 128], F32)
        ident_b = sb("ident_b", [128, 128], BF16)
        amask_b = sb("amask_b", [128, 512], BF16)
        ada_fm = sb("ada_fm", [128, 96], F32)
        a12 = sb("a12", [128, 32], F32)
        c_bf = sb("c_bf", [128, 16], BF16)
        stt_ = sb("stats", [128, 64], F32)
        stA = [sb("stA%d" % i, [128, 32], F32) for i in range(2)]
        dg = [sb("dg%d" % i, [128, 128], F32) for i in range(4)]
        dgrs = [sb("dgr%d" % i, [128, 128], F32) for i in range(2)]
        comb = sb("comb", [128, 4, 16], F32)
        rts = [sb("rt%d" % i, [128, 96], F32) for i in range(2)]
        OFFA, OFFB, OFFC = 0, 20480, 36864
        PPB = OFFC + 69760
        pp = sb("pp", [128, PPB], U8)
        ps = st.enter_context(nc.psum_tensor("ps", [128, 4096], F32))

        def view(off, n, dt, pat=None, **kw):
            sz = 2 if dt == BF16 else 4
            v = pp[:, off:off + n * sz].bitcast(dt)
            if pat:
                v = v.rearrange(pat, **kw)
            return v

        K3 = "p (k t) -> p k t"
        hT = view(OFFA, 16 * HT, BF16, K3, k=16)
        attnT = view(OFFB, 8 * TB, BF16, K3, k=8)
        pmixT = view(OFFB + 8192, 8 * TB, BF16, K3, k=8)
        actb = [view(OFFB + i * 4096, 4 * TB, BF16, K3, k=4) for i in range(2)]
        stmp = [view(OFFB + 8192 + i * 2048, 512, F32) for i in range(4)]
        xnj = [view(OFFC + i * 4096, D, BF16) for i in range(2)]
        xt = [view(OFFC + 8192 + i * 8192, D, F32) for i in range(2)]
        qT = view(OFFC + 24576, 8 * TB, BF16, K3, k=8)
        kT2 = view(OFFC + 32768, 4 * HT, BF16, K3, k=4)
        Vt = view(OFFC + 37888, 5 * 512, BF16, "p (t g d) -> p t g d", t=5, g=4)
        ub = [view(OFFC + 43008 + i * 2176, 544, F32) for i in range(2)]
        sAB = [view(OFFC + 47360 + i * 2176, 544, F32) for i in range(2)]
        pooled = view(OFFC + 51712, 8 * TB, BF16, K3, k=8)
        Pb = [view(OFFC + 59904 + i * 2048, 4 * 256, BF16, K3, k=4) for i in range(2)]
        PTs = [view(OFFC + 64000 + i * 2048, 1024, BF16) for i in range(2)]
        siga = view(OFFC, 16 * TB, BF16, K3, k=16)
        sigp_lo = view(OFFC + 16384, 8 * TB, BF16, K3, k=8)
        sigp_hi = view(OFFC + 43008, 8 * TB, BF16, K3, k=8)
        mergedT = view(OFFC + 24576, 16 * TB, BF16, K3, k=16)
        tA = [view(OFFC + 59904 + i * 2048, 512, F32) for i in range(2)]
        tB = [view(OFFC + 64000 + i * 2048, 512, F32) for i in range(2)]
        xn2j = [view(OFFC + i * 4096, D, BF16) for i in range(2)]
        h2Tf = [view(OFFC + 8192 + i * 8192, 16 * 128, F32, K3, k=16) for i in range(2)]
        sl = [view(OFFC + 24576 + i * 2048, 512, F32) for i in range(2)]
        h2T = view(OFFC + 28672, 16 * TB, BF16, K3, k=16)
        fjunk = view(OFFC + 45056, D, BF16)
        bc2 = view(OFFC + 49152, D, F32)

        sems = {e: st.enter_context(nc.semaphore("s_" + e)) for e in Prog.ENGS}
        dnames = ["w%d" % i for i in range(NSLOT)] + ["xt0", "xt1", "cst", "kdup", "bc2", "bcs", "bcl"] + \
                 ["xa%d" % i for i in range(4)] + ["out%d" % i for i in range(4)]
        dsem = {k: st.enter_context(nc.semaphore("d_" + k)) for k in dnames}

        t_w = [T() for _ in range(NSLOT)]
        t_pb = [T() for _ in range(8)]
        t_cst, t_id, t_ada, t_a12, t_cbf, t_bc, t_comb, t_ada2, t_a2, t_ada2a = [T() for _ in range(10)]
        t_dg = [T() for _ in range(4)]
        t_xa = [T() for _ in range(4)]
        t_hT = [T() for _ in range(5)]
        t_h2T = [T() for _ in range(4)]
        t_attn = [T() for _ in range(4)]
        t_pmix = T()
        t_act = [T(), T()]
        t_stmp = [T() for _ in range(4)]
        t_xnj, t_xt = [T(), T()], [T(), T()]
        t_dgr = [T(), T()]
        t_q, t_kn, t_kd, t_v = T(), T(), T(), T()
        t_ub, t_s = [T(), T()], [T(), T()]
        t_pooled = [T() for _ in range(8)]
        t_P, t_PT = [T(), T()], [T(), T()]
        t_sa = [T(), T()]
        t_merged = [T() for _ in range(4)]
        t_siga, t_sigp = [T() for _ in range(4)], [T() for _ in range(4)]
        t_tA, t_tB = [T(), T()], [T(), T()]
        t_xn2j, t_h2Tf, t_sl = [T(), T()], [T(), T()], [T(), T()]
        t_st = [T() for _ in range(8)]
        t_rts = [T(), T()]
        regA_M = t_hT
        regA_E = []
        regB_M = t_attn + [t_pmix]
        regB_E = t_act + t_stmp
        regC_M1_rest = [t_q, t_kn, t_kd, t_v] + t_ub + t_s + t_pooled + t_P + t_PT
        regC_M1 = t_xnj + t_xt + regC_M1_rest
        regC_M2 = t_merged + t_siga + t_sigp + t_tA + t_tB
        t_fj, t_bc2 = T(), T()
        t_gsc = [T(), T()]
        regC_E = t_xn2j + t_h2Tf + t_sl + t_h2T + [t_fj, t_bc2]

        state = {"bank": 0, "ev": 0, "resv": set(), "live": set()}

        def bank(n=1):
            b0 = state["bank"]
            for off in range(0, 16):
                b = (b0 + off) % 8
                if n > 1 and b % n:
                    continue
                if b + n > 8:
                    continue
                if any((b + i) in state["resv"] or (b + i) in state["live"] for i in range(n)):
                    continue
                for i in range(n):
                    state["live"].add(b + i)
                state["bank"] = (b + n) % 8
                return b
            raise AssertionError("out of PSUM banks: live=%s resv=%s" % (state["live"], state["resv"]))

        def bfree(b, n=1):
            for i in range(n):
                state["live"].discard(b + i)

        def pbank(b, n=1, c0=0, c1=None):
            if c1 is None:
                c1 = 512 * n
            return ps[:, b * 512 + c0:b * 512 + c1]

        def cs(c0, n):
            return cst[:, c0:c0 + n]

        def kcols(w2d, c0, ncols, k0, nk):
            src = w2d.rearrange("(k p) c -> p k c", p=128)[:, k0:k0 + nk, c0:c0 + ncols]
            return [(lambda sl_: sl_[:, 0:nk * ncols].rearrange(K3, k=nk), src)]

        def all_loads():
            for i in range(ADA_PRO):
                yield ("ada", kcols(w_ada, i * 256, 256, 0, 16))
            ada_i = [ADA_PRO]

            def ada_more(n=1):
                out = []
                for _ in range(n):
                    if ada_i[0] < 48:
                        out.append(("ada", kcols(w_ada, ada_i[0] * 256, 256, 0, 16)))
                        ada_i[0] += 1
                return out

            for blk in range(NB):
                for i in range(4):
                    yield ("q", kcols(w_in, i * 256, 256, 0, 16))
                yield ("k", kcols(w_in, 1024, 256, 0, 16))
                yield ("v", kcols(w_in, 1280, 256, 0, 16))
                for i in range(4):
                    yield ("u", kcols(w_in, 1536 + i * 256, 256, 0, 16))
                for gi in range(16):
                    yield ("g", kcols(w_in, 2560 + (gi // 8) * 2048 + (gi % 8) * 256, 256, 0, 16))
                    yield from ada_more()
                yield ("wp", [(lambda sl_: sl_[:, 0:2048].rearrange("p (g k d) -> p g k d", g=4, k=2),
                               w_pool.rearrange("g (k p) d -> p g k d", p=128))])
                yield from ada_more()
                for og in range(4):
                    yield ("brA", kcols(w_ab, og * 512, 512, 0, 8))
                    yield ("brB", kcols(w_pb, og * 512, 512, 0, 8))
                    yield from ada_more(2)
                for db in range(4):
                    for kh in range(2):
                        yield ("wo", kcols(w_out, db * 512, 512, kh * 8, 8))
                        yield from ada_more()
                for ex in range(NEXP):
                    for hh in range(2):
                        yield ("eg", kcols(w_eg[ex], hh * 256, 256, 0, 16))
                        yield ("eu", kcols(w_eu[ex], hh * 256, 256, 0, 16))
                    for dh in range(2):
                        yield ("ed", kcols(w_ed[ex], dh * 1024, 1024, 0, 4))

        specs = list(all_loads())
        wq = {"issued": 0, "released": 0, "acq": 0}

        def pump():
            while wq["issued"] < len(specs) and wq["issued"] < wq["released"] + NSLOT:
                i = wq["issued"]
                s = i % NSLOT
                for dfn, src in specs[i][1]:
                    d = dfn(wring[s])
                    P.op("pool", lambda e, d=d, src=src: e.dma_start(out=d, in_=src), writes=[t_w[s]],
                         dma="w%d" % s)
                wq["issued"] += 1

        def wnext(tag):
            i = wq["acq"]
            assert specs[i][0] == tag, (specs[i][0], tag, i)
            pump()
            assert wq["issued"] > i, "weight queue: too many held"
            wq["acq"] += 1
            return i % NSLOT

        def wrel(n=1):
            wq["released"] += n
            pump()

        def slot3(s, k):
            return wring[s][:, :].rearrange(K3, k=k)

        def evac(out_ap, in_ap, reads, writes, scale=None, same_ok=True):
            state["ev"] ^= 1
            if state["ev"]:
                if scale is None:
                    P.op("act", lambda e: e.activation(out=out_ap, in_=in_ap, func=AF.Copy),
                         reads=reads, writes=writes, same_ok=same_ok)
                else:
                    P.op("act", lambda e: e.activation(out=out_ap, in_=in_ap, func=AF.Copy, scale=scale),
                         reads=reads, writes=writes, same_ok=same_ok)
            else:
                if scale is None:
                    P.op("dve", lambda e: e.tensor_copy(out=out_ap, in_=in_ap),
                         reads=reads, writes=writes, same_ok=same_ok)
                else:
                    P.op("dve", lambda e: e.tensor_scalar(out=out_ap, in0=in_ap, scalar1=scale, scalar2=None,
                                                         op0=ALU.mult),
                         reads=reads, writes=writes, same_ok=same_ok)

        def mm(out_ap, pairs, reads, btiles, start=True, stop=True):
            n = len(pairs)
            for i, (l, r) in enumerate(pairs):
                P.op("pe", lambda e, l=l, r=r, i=i: e.matmul(out_ap, lhsT=l, rhs=r, start=(start and i == 0),
                                                            stop=(stop and i == n - 1)),
                     reads=reads, writes=btiles)

        P.op("sp", lambda e: e.dma_start(out=cst[:, :], in_=cst_d), writes=[t_cst], dma="cst")
        pump()
        P.op("pool", lambda e: e.memset(ident_f[:, :], 0.0), writes=[t_id])
        P.op("pool", lambda e: e.affine_select(out=ident_f[:, :], in_=ident_f[:, :], pattern=[[-1, 128]],
                                              compare_op=ALU.not_equal, fill=1.0, base=0,
                                              channel_multiplier=1), reads=[t_id], writes=[t_id])
        P.op("pool", lambda e: e.memset(ones_f[:, :], 1.0), writes=[t_id], same_ok=True)
        cs_eps = stt_[:, 28:29]
        P.op("pool", lambda e: e.memset(stt_[:, 28:29], EPS), writes=[t_id], same_ok=True)
        P.op("dve", lambda e: e.tensor_copy(out=c_bf[:, :], in_=cs(C_C, 16)), reads=[t_cst], writes=[t_cbf])
        P.op("dve", lambda e: e.tensor_copy(out=ident_b[:, :], in_=ident_f[:, :]), reads=[t_id], writes=[t_id])
        P.op("dve", lambda e: e.tensor_copy(out=amask_b[:, :], in_=cs(C_AM, 512)), reads=[t_cst, t_id],
             writes=[t_id])

        bA = bank()
        bfree(bA)
        state["resv"].add(bA)
        ada_n = [0]

        def ada_consume(n=1):
            for _ in range(n):
                if ada_n[0] >= 48:
                    return
                s_i = ada_n[0]
                ada_n[0] += 1
                s = wnext("ada")
                w3 = slot3(s, 16)
                for nn in range(2):
                    j = s_i * 2 + nn
                    mm(ps[:, bA * 512 + j:bA * 512 + j + 1],
                       [(w3[:, k, nn * 128:(nn + 1) * 128], c_bf[:, k:k + 1]) for k in range(16)],
                       [t_w[s], t_cbf], [t_pb[bA]])
                wrel()

        ada_consume(ADA_PRO)
        P.op("dve", lambda e: e.tensor_tensor(out=ada_fm[:, 0:32], in0=ps[:, bA * 512:bA * 512 + 32],
                                             in1=cs(C_BADA, 32), op=ALU.add),
             reads=[t_pb[bA], t_cst], writes=[t_ada])
        P.op("dve", lambda e: e.scalar_tensor_tensor(out=a12[:, 0:16], in0=ada_fm[:, 16:32], scalar=1.0,
                                                    in1=cs(C_G1, 16), op0=ALU.add, op1=ALU.mult),
             reads=[t_ada, t_cst], writes=[t_a12])

        def ada_finish_a():
            assert ada_n[0] >= 24
            P.op("dve", lambda e: e.tensor_tensor(out=ada_fm[:, 32:48], in0=ps[:, bA * 512 + 32:bA * 512 + 48],
                                                 in1=cs(C_BADA + 32, 16), op=ALU.add),
                 reads=[t_pb[bA], t_cst], writes=[t_ada2a])

        def ada_finish():
            assert ada_n[0] == 48
            P.op("dve", lambda e: e.tensor_tensor(out=ada_fm[:, 48:96], in0=ps[:, bA * 512 + 48:bA * 512 + 96],
                                                 in1=cs(C_BADA + 48, 48), op=ALU.add),
                 reads=[t_pb[bA], t_cst], writes=[t_ada2])
            P.op("dve", lambda e: e.scalar_tensor_tensor(out=a12[:, 16:32], in0=ada_fm[:, 64:80], scalar=1.0,
                                                        in1=cs(C_G2, 16), op0=ALU.add, op1=ALU.mult),
                 reads=[t_ada2, t_cst], writes=[t_a2])
            state["resv"].discard(bA)

        def regen_bc(vec_ap, vec_tiles, factor=1.0, dst=None, tdst=None):
            dst = bc if dst is None else dst
            tdst = t_bc if tdst is None else tdst
            for j4 in range(4):
                b = bank()
                for jj in range(4):
                    j = j4 * 4 + jj
                    P.op("dve", lambda e, j=j: e.tensor_scalar(out=dg[j % 4][:, :], in0=ident_f[:, :],
                                                              scalar1=vec_ap[:, j:j + 1], scalar2=factor,
                                                              op0=ALU.mult, op1=ALU.mult),
                         reads=[t_id] + vec_tiles, writes=[t_dg[j % 4]])
                    P.op("pe", lambda e, j=j, jj=jj, b=b: e.matmul(pbank(b, 1, jj * 128, jj * 128 + 128),
                                                                  lhsT=ones_f[:, :], rhs=dg[j % 4][:, :],
                                                                  start=True, stop=True),
                         reads=[t_id, t_dg[j % 4]], writes=[t_pb[b]])
                evac(dst[:, j4 * 512:(j4 + 1) * 512], pbank(b), [t_pb[b]], [tdst])
                bfree(b)

        def rms_stats(src_ap, src_tiles, junk_ap, junk_tiles, slot_i):
            g = t_st[slot_i]
            c = 32 + slot_i * 4
            P.op("act", lambda e: e.activation(out=junk_ap, in_=src_ap, func=AF.Square,
                                              accum_out=stt_[:, c:c + 1]),
                 reads=src_tiles, writes=junk_tiles + [g])
            P.op("act", lambda e: e.activation(out=stt_[:, c + 1:c + 2], in_=stt_[:, c:c + 1], func=AF.Sqrt,
                                              scale=1.0 / D, bias=cs_eps),
                 reads=[g, t_id], writes=[g])
            P.op("dve", lambda e: e.reciprocal(out=stt_[:, c + 2:c + 3], in_=stt_[:, c + 1:c + 2]),
                 reads=[g], writes=[g])
            return stt_[:, c + 2:c + 3], g

        def emit_M1a(r0):
            m1 = {}

            def m1_N1(ti):
                xb = xt[ti % 2]
                P.op("sp", lambda e, xb=xb, ti=ti, r0=r0: e.dma_start(
                    out=xb, in_=x_d[r0 + ti * 128:r0 + (ti + 1) * 128, :]),
                     writes=[t_xt[ti % 2]], dma="xt%d" % (ti % 2))
                rstd, g = rms_stats(xb, [t_xt[ti % 2]], xnj[ti % 2], [t_xnj[ti % 2]], ti % 2)
                dgr, tdgr = dgrs[ti % 2], t_dgr[ti % 2]
                P.op("dve", lambda e: e.tensor_scalar(out=dgr[:, :], in0=ident_f[:, :], scalar1=rstd,
                                                     scalar2=None, op0=ALU.mult),
                     reads=[t_id, g], writes=[tdgr])

            def m1_N2(ti):
                xb = xt[ti % 2]
                dgr, tdgr = dgrs[ti % 2], t_dgr[ti % 2]
                b4 = bank(4)
                m1[ti] = b4
                for c in range(16):
                    P.op("pe", lambda e, c=c: e.matmul(
                        ps[:, b4 * 512 + c * 128:b4 * 512 + (c + 1) * 128], lhsT=xb[:, c * 128:(c + 1) * 128],
                        rhs=dgr[:, :], start=True, stop=True),
                         reads=[t_xt[ti % 2], tdgr], writes=[t_pb[b4 + c // 4]])

            def m1_N3(ti):
                b4 = m1.pop(ti)
                for c in range(16):
                    src = ps[:, b4 * 512 + c * 128:b4 * 512 + (c + 1) * 128]
                    dst = hT[:, c, ti * 128:(ti + 1) * 128]
                    if c % 2 == 0:
                        P.op("act", lambda e, c=c, src=src, dst=dst: e.activation(
                            out=dst, in_=src, func=AF.Identity, scale=a12[:, c:c + 1], bias=ada_fm[:, c:c + 1]),
                             reads=[t_pb[b4 + c // 4], t_a12, t_ada], writes=[t_hT[ti]], same_ok=True)
                    else:
                        P.op("dve", lambda e, c=c, src=src, dst=dst: e.tensor_scalar(
                            out=dst, in0=src, scalar1=a12[:, c:c + 1], scalar2=ada_fm[:, c:c + 1],
                            op0=ALU.mult, op1=ALU.add),
                             reads=[t_pb[b4 + c // 4], t_a12, t_ada], writes=[t_hT[ti]], same_ok=True)
                bfree(b4, 4)

            def stage(k):
                if k == 0:
                    m1_N1(0)
                    m1_N1(1)
                else:
                    ti = k - 1
                    m1_N2(ti)
                    m1_N3(ti)
                    if ti + 2 < 5:
                        m1_N1(ti + 2)
            return stage


        for blk in range(NB):
            r0 = blk * TB
            if blk > 0:
                P.fence(regB_M, regB_E)
                P.fence(regC_M1_rest, regC_E + regC_M2)
            if blk == 0:
                st0 = emit_M1a(0)
                for k in range(6):
                    st0(k)

            hmain = lambda k: hT[:, k, 128:HT]
            for s_i in range(4):
                s = wnext("q")
                w3 = slot3(s, 16)
                for cc in range(2):
                    ch = s_i * 2 + cc
                    b = bank()
                    mm(pbank(b), [(w3[:, k, cc * 128:(cc + 1) * 128], hmain(k)) for k in range(16)],
                       [t_w[s]] + t_hT, [t_pb[b]])
                    evac(qT[:, ch, :], pbank(b), [t_pb[b]], [t_q], scale=0.125)
                    bfree(b)
                wrel()
            s = wnext("k")
            w3 = slot3(s, 16)
            for ch in range(2):
                b = bank()
                mm(pbank(b), [(w3[:, k, ch * 128:(ch + 1) * 128], hmain(k)) for k in range(16)],
                   [t_w[s]] + t_hT, [t_pb[b]])
                b_h = bank()
                mm(pbank(b_h, 1, 0, 128), [(w3[:, k, ch * 128:(ch + 1) * 128], hT[:, k, 0:128]) for k in range(16)],
                   [t_w[s]] + t_hT, [t_pb[b_h]])
                for hf_ in range(2):
                    kv = ch * 2 + hf_
                    o = hf_ * 64
                    evac(kT2[o:o + 64, kv, 128:HT], ps[o:o + 64, b * 512:b * 512 + 512], [t_pb[b]], [t_kn])
                    evac(kT2[o:o + 64, kv, 0:128], ps[o:o + 64, b_h * 512:b_h * 512 + 128], [t_pb[b_h]], [t_kn])
                bfree(b)
                bfree(b_h)
            wrel()
            for kv in range(4):
                o = (kv % 2) * 64
                P.op("sp", lambda e, kv=kv, o=o: e.dma_start(out=kT2[64 - o:128 - o, kv, :], in_=kT2[o:o + 64, kv, :]),
                     reads=[t_kn], writes=[t_kd], dma="kdup")
            s = wnext("v")
            w3 = slot3(s, 16)
            for ti in range(5):
                b = bank()
                mm(pbank(b, 1, 0, 256), [(hT[:, k, ti * 128:(ti + 1) * 128], w3[:, k, :]) for k in range(16)],
                   [t_w[s]] + t_hT, [t_pb[b]])
                src = pbank(b, 1, 0, 256).rearrange("p (g d) -> p g d", g=4)
                evac(Vt[:, ti, :, 0:64], src, [t_pb[b]], [t_v])
                evac(Vt[:, ti, :, 64:128], src, [t_pb[b]], [t_v])
                bfree(b)
            wrel()

            att_state = {}

            def att_A(i):
                qt, kv = divmod(i, 4)
                mcol = 256 if (blk == 0 and qt == 0) else 0
                b2 = bank(2)
                for j in range(4):
                    h = kv * 4 + j
                    off = (h % 2) * 64
                    o_ap = ps[:, b2 * 512 + j * 256:b2 * 512 + (j + 1) * 256]
                    P.op("pe", lambda e, o_ap=o_ap, off=off, h=h, qt=qt, kv=kv: e.matmul(
                        o_ap, lhsT=qT[off:off + 64, h // 2, qt * 128:(qt + 1) * 128],
                        rhs=kT2[off:off + 64, kv, qt * 128:(qt + 2) * 128], start=True, stop=False),
                         reads=[t_q, t_kn, t_kd], writes=[t_pb[b2 + j // 2]])
                    P.op("pe", lambda e, o_ap=o_ap, mcol=mcol: e.matmul(
                        o_ap, lhsT=ident_b[:, :], rhs=amask_b[:, mcol:mcol + 256], start=False, stop=True),
                         reads=[t_id], writes=[t_pb[b2 + j // 2]])
                att_state[i] = b2

            def att_ctx(i):
                qt, kv = divmod(i, 4)
                return dict(qt=qt, kv=kv, S=stA[i % 2], tS=t_sa[i % 2], Pn=Pb[i % 2], tP=t_P[i % 2],
                            PTb=PTs[i % 2], tPT=t_PT[i % 2], sink4=cs(C_SINK + kv * 4, 4))

            def att_B1(i):
                c = att_ctx(i)
                S, tS, sink4 = c["S"], c["tS"], c["sink4"]
                b2 = att_state[i]
                pbs = [t_pb[b2], t_pb[b2 + 1]]
                P.op("dve", lambda e: e.tensor_reduce(out=S[:, 0:4], in_=pbank(b2, 2).rearrange(K3, k=4), axis=AX.X,
                                                     op=ALU.max), reads=pbs, writes=[tS])
                P.op("dve", lambda e: e.tensor_tensor(out=S[:, 0:4], in0=S[:, 0:4], in1=sink4, op=ALU.max),
                     reads=[tS, t_cst], writes=[tS])
                P.op("dve", lambda e: e.tensor_scalar(out=S[:, 4:8], in0=S[:, 0:4], scalar1=-1.0, scalar2=None,
                                                     op0=ALU.mult), reads=[tS], writes=[tS])
                P.op("dve", lambda e: e.tensor_tensor(out=S[:, 8:12], in0=sink4, in1=S[:, 0:4], op=ALU.subtract),
                     reads=[tS, t_cst], writes=[tS], same_ok=True)

            def att_B2(i):
                c = att_ctx(i)
                S, tS, Pn, tP = c["S"], c["tS"], c["Pn"], c["tP"]
                b2 = att_state.pop(i)
                pbs = [t_pb[b2], t_pb[b2 + 1]]
                for j in range(4):
                    P.op("act", lambda e, j=j: e.activation(
                        out=Pn[:, j, :], in_=ps[:, b2 * 512 + j * 256:b2 * 512 + (j + 1) * 256], func=AF.Exp,
                        bias=S[:, 4 + j:5 + j], scale=1.0, accum_out=S[:, 12 + j:13 + j]),
                         reads=pbs + [tS], writes=[tP, tS], same_ok=True)
                P.op("act", lambda e: e.activation(out=S[:, 16:20], in_=S[:, 8:12], func=AF.Exp),
                     reads=[tS], writes=[tS], same_ok=True)
                bfree(b2, 2)

            def att_B3(i):
                c = att_ctx(i)
                S, tS, Pn, tP = c["S"], c["tS"], c["Pn"], c["tP"]
                P.op("dve", lambda e: e.tensor_tensor(out=S[:, 20:24], in0=S[:, 12:16], in1=S[:, 16:20], op=ALU.add),
                     reads=[tS], writes=[tS])
                P.op("dve", lambda e: e.reciprocal(out=S[:, 24:28], in_=S[:, 20:24]), reads=[tS], writes=[tS])
                P.op("dve", lambda e: e.tensor_tensor(
                    out=Pn[:, :, :], in0=Pn[:, :, :], in1=S[:, 24:28].unsqueeze(2).broadcast_to([128, 4, 256]),
                    op=ALU.mult), reads=[tP, tS], writes=[tP])

            def att_B4(i):
                c = att_ctx(i)
                Pn, tP, PTb, tPT = c["Pn"], c["tP"], c["PTb"], c["tPT"]
                bt = bank()
                ptv = pbank(bt).bitcast(BF16)
                for half in range(2):
                    for j in range(4):
                        o = (half * 4 + j) * 128
                        P.op("pe", lambda e, o=o, j=j, half=half: e.transpose(
                            out=ptv[:, o:o + 128], in_=Pn[:, j, half * 128:(half + 1) * 128],
                            identity=ident_b[:, :]), reads=[tP, t_id], writes=[t_pb[bt]])
                evac(PTb, ptv, [t_pb[bt]], [tPT])
                bfree(bt)

            def att_B5(i):
                c = att_ctx(i)
                qt, kv, PTb, tPT = c["qt"], c["kv"], c["PTb"], c["tPT"]
                bo = bank()
                mm(pbank(bo), [(Vt[:, qt, kv, :], PTb[:, 0:512]), (Vt[:, qt + 1, kv, :], PTb[:, 512:1024])],
                   [t_v, tPT], [t_pb[bo]])
                for j in range(4):
                    h = kv * 4 + j
                    o2 = (h % 2) * 64
                    srcp = ps[o2:o2 + 64, bo * 512 + j * 128:bo * 512 + (j + 1) * 128]
                    dstp = attnT[o2:o2 + 64, h // 2, qt * 128:(qt + 1) * 128]
                    if j % 2 == 0:
                        P.op("act", lambda e, srcp=srcp, dstp=dstp: e.activation(out=dstp, in_=srcp, func=AF.Copy),
                             reads=[t_pb[bo]], writes=[t_attn[qt]], same_ok=True)
                    else:
                        P.op("dve", lambda e, srcp=srcp, dstp=dstp: e.tensor_copy(out=dstp, in_=srcp),
                             reads=[t_pb[bo]], writes=[t_attn[qt]], same_ok=True)
                bfree(bo)

            def u_chunk(s, n_in, ch):
                w3 = slot3(s, 16)
                g = ch // 2
                wdw = 2 << g
                u = ub[ch % 2]
                tu = t_ub[ch % 2]
                b = bank()
                mm(pbank(b), [(w3[:, k, n_in * 128:(n_in + 1) * 128], hmain(k)) for k in range(16)],
                   [t_w[s]] + t_hT, [t_pb[b]])
                P.op("act", lambda e: e.activation(out=u[:, 16:528], in_=pbank(b), func=AF.Copy),
                     reads=[t_pb[b]], writes=[tu])
                bfree(b)
                b2 = bank()
                mm(pbank(b2, 1, 0, 16), [(w3[:, k, n_in * 128:(n_in + 1) * 128], hT[:, k, 112:128]) for k in range(16)],
                   [t_w[s]] + t_hT, [t_pb[b2]])
                if blk == 0:
                    P.op("dve", lambda e: e.tensor_scalar(out=u[:, 0:16], in0=pbank(b2, 1, 0, 16),
                                                         scalar1=cs(C_UF, 1), scalar2=None, op0=ALU.mult),
                         reads=[t_pb[b2], t_cst], writes=[tu], same_ok=True)
                    bfree(b2)
                else:
                    P.op("dve", lambda e: e.tensor_copy(out=u[:, 0:16], in_=pbank(b2, 1, 0, 16)),
                         reads=[t_pb[b2]], writes=[tu], same_ok=True)
                bfree(b2)
                cur, tcur = u, tu
                step = 1
                i = 0
                while step < wdw:
                    nxt, tn = sAB[i % 2], t_s[i % 2]
                    P.op("pool", lambda e, cur=cur, nxt=nxt, step=step: e.tensor_tensor(
                        out=nxt[:, step:528], in0=cur[:, step:528], in1=cur[:, 0:528 - step], op=ALU.add),
                         reads=[tcur], writes=[tn])
                    cur, tcur = nxt, tn
                    step *= 2
                    i += 1
                P.op("dve", lambda e, cur=cur: e.scalar_tensor_tensor(
                    out=pooled[:, ch, :], in0=cur[:, 16:528], scalar=1.0 / wdw, in1=u[:, 16:528],
                    op0=ALU.mult, op1=ALU.subtract),
                     reads=[tcur, tu], writes=[t_pooled[ch]])
                if blk == 0:
                    oth, toth = sAB[i % 2], t_s[i % 2]
                    P.op("dve", lambda e, cur=cur, oth=oth: e.tensor_tensor(
                        out=oth[:, 0:16], in0=cur[:, 16:32], in1=cs(C_INVC + g * 16, 16), op=ALU.mult),
                         reads=[tcur, t_cst], writes=[toth])
                    P.op("dve", lambda e, oth=oth: e.tensor_tensor(
                        out=pooled[:, ch, 0:16], in0=oth[:, 0:16], in1=u[:, 16:32], op=ALU.subtract),
                         reads=[toth, tu], writes=[t_pooled[ch]])

            u_slot = None
            for ch in range(8):
                if ch % 2 == 0:
                    u_slot = wnext("u")
                u_chunk(u_slot, ch % 2, ch)
                if ch % 2 == 1:
                    wrel()
            P.fence(t_siga + t_sigp[0:2], t_xnj + t_xt)
            P.fence(t_sigp[2:4], t_ub + t_s)

            def gate_unit(gi):
                which, hh = divmod(gi, 8)
                s = wnext("g")
                w3 = slot3(s, 16)
                for nn in range(2):
                    ch = hh * 2 + nn
                    b = bank()
                    mm(pbank(b), [(w3[:, k, nn * 128:(nn + 1) * 128], hmain(k)) for k in range(16)],
                       [t_w[s]] + t_hT, [t_pb[b]])
                    if which == 0:
                        dst, tdst = siga[:, ch, :], t_siga[ch // 4]
                    elif ch < 8:
                        dst, tdst = sigp_lo[:, ch, :], t_sigp[ch // 4]
                    else:
                        dst, tdst = sigp_hi[:, ch - 8, :], t_sigp[ch // 4]
                    P.op("act", lambda e, b=b, dst=dst: e.activation(out=dst, in_=pbank(b), func=AF.Tanh, scale=0.5),
                         reads=[t_pb[b]], writes=[tdst], same_ok=True)
                    bfree(b)
                wrel()
                ada_consume()

            for sst in range(16 + 3):
                if sst < 16:
                    att_A(sst)
                    gate_unit(sst)
                if sst == 10:
                    if blk == 0:
                        ada_finish_a()
                    regen_bc(ada_fm[:, 32:48], [t_ada2a], factor=0.5)
                if 0 <= sst - 2 < 16:
                    att_B3(sst - 2)
                if 0 <= sst - 1 < 16:
                    att_B1(sst - 1)
                    att_B2(sst - 1)
                if 0 <= sst - 2 < 16:
                    att_B4(sst - 2)
                if 0 <= sst - 3 < 16:
                    att_B5(sst - 3)

            s = wnext("wp")
            wp4 = wring[s][:, 0:2048].rearrange("p (g k d) -> p g k d", g=4, k=2)
            for g in range(4):
                for oc in range(2):
                    b = bank()
                    mm(pbank(b), [(wp4[:, g, kc, oc * 128:(oc + 1) * 128], pooled[:, g * 2 + kc, :]) for kc in range(2)],
                       [t_w[s]] + t_pooled, [t_pb[b]])
                    ch = g * 2 + oc
                    P.op("act", lambda e, b=b, ch=ch: e.activation(out=pmixT[:, ch, :], in_=pbank(b), func=AF.Identity,
                                                                  scale=cs(C_PSC + ch, 1), bias=0.0),
                         reads=[t_pb[b], t_cst], writes=[t_pmix], same_ok=True)
                    bfree(b)
            wrel()
            ada_consume()

            P.fence(t_merged, [t_q, t_kn, t_kd, t_v])
            P.fence(t_tA + t_tB, t_P + t_PT)
            for og in range(4):
                sA_ = wnext("brA")
                sB_ = wnext("brB")
                wa3, wb3 = slot3(sA_, 8), slot3(sB_, 8)
                for n in range(4):
                    ba = bank()
                    mm(pbank(ba), [(wa3[:, kc, n * 128:(n + 1) * 128], attnT[:, kc, :]) for kc in range(8)],
                       [t_w[sA_]] + t_attn, [t_pb[ba]])
                    bb = bank()
                    mm(pbank(bb), [(wb3[:, kc, n * 128:(n + 1) * 128], pmixT[:, kc, :]) for kc in range(8)],
                       [t_w[sB_], t_pmix], [t_pb[bb]])
                    i2 = n % 2
                    ch = og * 4 + n
                    ga_ap = siga[:, ch, :]
                    gp_ap = sigp_lo[:, ch, :] if ch < 8 else sigp_hi[:, ch - 8, :]
                    P.op("dve", lambda e, ba=ba, ga_ap=ga_ap, i2=i2: e.scalar_tensor_tensor(
                        out=tA[i2], in0=ga_ap, scalar=1.0, in1=pbank(ba), op0=ALU.add, op1=ALU.mult),
                         reads=[t_pb[ba], t_siga[og]], writes=[t_tA[i2]])
                    P.op("dve", lambda e, bb=bb, gp_ap=gp_ap, i2=i2: e.scalar_tensor_tensor(
                        out=tB[i2], in0=gp_ap, scalar=1.0, in1=pbank(bb), op0=ALU.add, op1=ALU.mult),
                         reads=[t_pb[bb], t_sigp[og]], writes=[t_tB[i2]])
                    bfree(ba)
                    bfree(bb)
                    P.op("pool", lambda e, og=og, n=n, i2=i2: e.tensor_tensor(out=mergedT[:, og * 4 + n, :], in0=tA[i2],
                                                                             in1=tB[i2], op=ALU.add),
                         reads=[t_tA[i2], t_tB[i2]], writes=[t_merged[og]], same_ok=True)
                wrel(2)
                ada_consume(2)

            for ti in range(4):
                P.op("sp", lambda e, ti=ti, r0=r0: e.dma_start(
                    out=xacc[:, ti, :], in_=x_d[r0 + 128 + ti * 128:r0 + 128 + (ti + 1) * 128, :]),
                     writes=[t_xa[ti]], dma="xa%d" % ti)
            k_ev = 0
            for db in range(4):
                bks = [bank() for _ in range(4)]
                for kh in range(2):
                    s = wnext("wo")
                    w3 = slot3(s, 8)
                    for ti in range(4):
                        mm(pbank(bks[ti]), [(mergedT[:, kh * 8 + k, ti * 128:(ti + 1) * 128], w3[:, k, :])
                                            for k in range(8)],
                           [t_w[s]] + t_merged, [t_pb[bks[ti]]], start=(kh == 0), stop=(kh == 1))
                    wrel()
                    ada_consume()
                for ti in range(4):
                    b = bks[ti]
                    i4 = k_ev % 2
                    k_ev += 1
                    P.op("dve", lambda e, b=b, db=db, i4=i4: e.tensor_tensor(out=tA[i4], in0=pbank(b),
                                                                            in1=bc[:, db * 512:(db + 1) * 512],
                                                                            op=ALU.mult),
                         reads=[t_pb[b], t_bc], writes=[t_tA[i4]])
                    bfree(b)
                    P.op("pool", lambda e, ti=ti, db=db, i4=i4: e.tensor_tensor(
                        out=xacc[:, ti, db * 512:(db + 1) * 512], in0=xacc[:, ti, db * 512:(db + 1) * 512],
                        in1=tA[i4], op=ALU.add),
                         reads=[t_tA[i4]], writes=[t_xa[ti]], same_ok=True)

            if blk == 0:
                ada_finish()
            P.fence(regB_E, regB_M)
            P.fence(regC_E, regC_M2)
            n2 = {}

            def n2_N1(ti):
                rstd, g = rms_stats(xacc[:, ti, :], [t_xa[ti]], xn2j[ti % 2], [t_xn2j[ti % 2]], 4 + ti % 2)
                dgr, tdgr = dgrs[ti % 2], t_dgr[ti % 2]
                P.op("dve", lambda e: e.tensor_scalar(out=dgr[:, :], in0=ident_f[:, :], scalar1=rstd,
                                                     scalar2=None, op0=ALU.mult),
                     reads=[t_id, g], writes=[tdgr])

            def n2_N2(ti):
                dgr, tdgr = dgrs[ti % 2], t_dgr[ti % 2]
                b4 = bank(4)
                n2[ti] = b4
                for c in range(16):
                    P.op("pe", lambda e, c=c: e.matmul(
                        ps[:, b4 * 512 + c * 128:b4 * 512 + (c + 1) * 128],
                        lhsT=xacc[:, ti, c * 128:(c + 1) * 128], rhs=dgr[:, :], start=True, stop=True),
                         reads=[t_xa[ti], tdgr], writes=[t_pb[b4 + c // 4]])

            def n2_N3(ti):
                b4 = n2.pop(ti)
                hf, thf = h2Tf[ti % 2], t_h2Tf[ti % 2]
                for c in range(16):
                    src = ps[:, b4 * 512 + c * 128:b4 * 512 + (c + 1) * 128]
                    if c % 2 == 0:
                        P.op("act", lambda e, c=c, src=src: e.activation(
                            out=hf[:, c, :], in_=src, func=AF.Identity, scale=a12[:, 16 + c:17 + c],
                            bias=ada_fm[:, 48 + c:49 + c]),
                             reads=[t_pb[b4 + c // 4], t_a2, t_ada2], writes=[thf], same_ok=True)
                    else:
                        P.op("dve", lambda e, c=c, src=src: e.tensor_scalar(
                            out=hf[:, c, :], in0=src, scalar1=a12[:, 16 + c:17 + c],
                            scalar2=ada_fm[:, 48 + c:49 + c], op0=ALU.mult, op1=ALU.add),
                             reads=[t_pb[b4 + c // 4], t_a2, t_ada2], writes=[thf], same_ok=True)
                bfree(b4, 4)
                P.op("pool", lambda e: e.tensor_copy(out=h2T[:, :, ti * 128:(ti + 1) * 128], in_=hf),
                     reads=[thf], writes=[t_h2T[ti]])

            def n2_R(ti):
                hf, thf = h2Tf[ti % 2], t_h2Tf[ti % 2]
                rtb, trt = rts[ti % 2], t_rts[ti % 2]
                br = bank()
                wr3 = cs(C_WR, 320).rearrange(K3, k=16)
                mm(pbank(br, 1, 0, 20), [(hf[:, k, :], wr3[:, k, :]) for k in range(16)], [thf, t_cst], [t_pb[br]])
                P.op("dve", lambda e: e.tensor_tensor(out=rtb[:, 0:20], in0=pbank(br, 1, 0, 20), in1=cs(C_BR, 20),
                                                     op=ALU.add), reads=[trt, t_pb[br], t_cst], writes=[trt])
                bfree(br)

            def router_ops(ti):
                rtb, trt = rts[ti % 2], t_rts[ti % 2]
                R = lambda a_, b_: rtb[:, a_:b_]
                ops = []
                ro = lambda fn, rd=(): ops.append(lambda: P.op("dve", fn, reads=[trt] + list(rd), writes=[trt]))
                ra = lambda fn: ops.append(lambda: P.op("act", fn, reads=[trt], writes=[trt]))

                ro(lambda e: e.tensor_reduce(out=R(20, 21), in_=R(0, 4), axis=AX.X, op=ALU.max))
                ro(lambda e: e.tensor_scalar(out=R(21, 22), in0=R(20, 21), scalar1=-1.0, scalar2=None, op0=ALU.mult))
                ra(lambda e: e.activation(out=R(24, 28), in_=R(0, 4), func=AF.Exp, bias=R(21, 22), scale=1.0,
                                          accum_out=R(22, 23)))
                ro(lambda e: e.reciprocal(out=R(23, 24), in_=R(22, 23)))
                ro(lambda e: e.tensor_scalar(out=R(28, 32), in0=R(0, 4), scalar1=R(20, 21), scalar2=None,
                                             op0=ALU.is_equal))
                ro(lambda e: e.tensor_tensor(out=R(32, 48).rearrange("p (g e) -> p g e", g=4),
                                             in0=R(4, 20).rearrange("p (g e) -> p g e", g=4),
                                             in1=R(28, 32).unsqueeze(2).broadcast_to([128, 4, 4]), op=ALU.mult))
                ro(lambda e: e.tensor_reduce(out=R(48, 52), in_=R(32, 48).rearrange("p (g e) -> p e g", g=4),
                                             axis=AX.X, op=ALU.add))
                ro(lambda e: e.tensor_reduce(out=R(52, 53), in_=R(48, 52), axis=AX.X, op=ALU.max))
                ro(lambda e: e.tensor_scalar(out=R(56, 60), in0=R(48, 52), scalar1=R(52, 53), scalar2=None,
                                             op0=ALU.is_equal))
                ro(lambda e: e.scalar_tensor_tensor(out=R(60, 64), in0=R(56, 60), scalar=NEG, in1=R(48, 52),
                                                    op0=ALU.mult, op1=ALU.add))
                ro(lambda e: e.tensor_reduce(out=R(53, 54), in_=R(60, 64), axis=AX.X, op=ALU.max))
                ro(lambda e: e.tensor_scalar(out=R(64, 68), in0=R(60, 64), scalar1=R(53, 54), scalar2=None,
                                             op0=ALU.is_equal))
                ro(lambda e: e.tensor_tensor(out=R(54, 55), in0=R(53, 54), in1=R(52, 53), op=ALU.subtract))
                ra(lambda e: e.activation(out=R(55, 56), in_=R(54, 55), func=AF.Exp))
                ro(lambda e: e.tensor_scalar(out=R(68, 69), in0=R(55, 56), scalar1=1.0, scalar2=None, op0=ALU.add))
                ro(lambda e: e.reciprocal(out=R(69, 70), in_=R(68, 69)))
                ro(lambda e: e.tensor_tensor(out=R(70, 71), in0=R(55, 56), in1=R(69, 70), op=ALU.mult))
                ro(lambda e: e.tensor_tensor(out=R(71, 72), in0=R(69, 70), in1=R(23, 24), op=ALU.mult))
                ro(lambda e: e.tensor_tensor(out=R(72, 73), in0=R(70, 71), in1=R(23, 24), op=ALU.mult))
                ro(lambda e: e.tensor_scalar(out=R(76, 80), in0=R(56, 60), scalar1=R(71, 72), scalar2=None,
                                             op0=ALU.mult))
                ro(lambda e: e.scalar_tensor_tensor(out=R(80, 84), in0=R(64, 68), scalar=R(72, 73), in1=R(76, 80),
                                                    op0=ALU.mult, op1=ALU.add))
                ops.append(lambda: P.op("dve", lambda e: e.tensor_tensor(
                    out=comb[:, ti, :].rearrange("p (g e) -> p g e", g=4),
                    in0=R(28, 32).unsqueeze(2).broadcast_to([128, 4, 4]),
                    in1=R(80, 84).unsqueeze(1).broadcast_to([128, 4, 4]), op=ALU.mult),
                    reads=[trt], writes=[t_comb], same_ok=True))
                return ops

            def router_pair(t0_, t1_):
                la, lb = router_ops(t0_), router_ops(t1_)
                for fa, fb in zip(la, lb):
                    fa()
                    fb()

            n2_N1(0)
            n2_N1(1)
            n2_N2(0)
            n2_N2(1)
            n2_N1(2)
            n2_N1(3)
            n2_N3(0)
            n2_R(0)
            n2_N2(2)
            n2_N3(1)
            n2_R(1)
            router_pair(0, 1)
            n2_N2(3)
            n2_N3(2)
            n2_R(2)
            n2_N3(3)
            n2_R(3)
            router_pair(2, 3)

            regen_bc(ada_fm[:, 80:96], [t_ada2])
            kst = 0
            for ex in range(NEXP):
                if ex == HOIST_AT:
                    P.op("sp", lambda e: e.dma_start(out=bc2, in_=fg_d), writes=[t_bc2], dma="bc2")
                    if blk + 1 < NB:
                        P.fence(t_xnj + t_xt, t_xn2j + t_h2Tf)
                        hoist_stage = emit_M1a((blk + 1) * TB)
                if blk + 1 < NB and HOIST_AT <= ex < HOIST_AT + 6:
                    hoist_stage(ex - HOIST_AT)
                ab, tab = actb[ex % 2], t_act[ex % 2]
                for hh in range(2):
                    sg_ = wnext("eg")
                    su_ = wnext("eu")
                    wg3, wu3 = slot3(sg_, 16), slot3(su_, 16)
                    for ff in range(2):
                        fc = hh * 2 + ff
                        bg = bank()
                        mm(pbank(bg), [(wg3[:, k, ff * 128:(ff + 1) * 128], h2T[:, k, :]) for k in range(16)],
                           [t_w[sg_]] + t_h2T, [t_pb[bg]])
                        bu = bank()
                        mm(pbank(bu), [(wu3[:, k, ff * 128:(ff + 1) * 128], h2T[:, k, :]) for k in range(16)],
                           [t_w[su_]] + t_h2T, [t_pb[bu]])
                        i2 = fc % 2
                        P.op("act", lambda e, bg=bg, i2=i2: e.activation(out=sl[i2], in_=pbank(bg), func=AF.Silu),
                             reads=[t_pb[bg]], writes=[t_sl[i2]])
                        P.op("dve", lambda e, bu=bu, i2=i2, ab=ab, fc=fc: e.tensor_tensor(out=ab[:, fc, :], in0=sl[i2],
                                                                                         in1=pbank(bu), op=ALU.mult),
                             reads=[t_sl[i2], t_pb[bu]], writes=[tab], same_ok=True)
                        bfree(bg)
                        bfree(bu)
                    wrel(2)
                for dh in range(2):
                    sd_ = wnext("ed")
                    wd3 = slot3(sd_, 4)
                    for ti in range(4):
                        for dd in range(2):
                            db = dh * 2 + dd
                            b = bank()
                            mm(pbank(b), [(ab[:, fc, ti * 128:(ti + 1) * 128], wd3[:, fc, dd * 512:(dd + 1) * 512])
                                          for fc in range(4)], [t_w[sd_], tab], [t_pb[b]])
                            i4 = kst % 4
                            kst += 1
                            P.op("dve", lambda e, b=b, ti=ti, ex=ex, db=db, i4=i4: e.scalar_tensor_tensor(
                                out=stmp[i4], in0=pbank(b), scalar=comb[:, ti, ex:ex + 1],
                                in1=bc[:, db * 512:(db + 1) * 512], op0=ALU.mult, op1=ALU.mult),
                                 reads=[t_pb[b], t_comb, t_bc], writes=[t_stmp[i4]])
                            bfree(b)
                            P.op("pool", lambda e, ti=ti, db=db, i4=i4: e.tensor_tensor(
                                out=xacc[:, ti, db * 512:(db + 1) * 512], in0=xacc[:, ti, db * 512:(db + 1) * 512],
                                in1=stmp[i4], op=ALU.add),
                                 reads=[t_stmp[i4]], writes=[t_xa[ti]], same_ok=True)
                    wrel()

            for ti in range(4):
                rstd, g = rms_stats(xacc[:, ti, :], [t_xa[ti]], fjunk, [t_fj], 6 + ti % 2)
                P.op("dve", lambda e, ti=ti, rstd=rstd: e.scalar_tensor_tensor(
                    out=xacc[:, ti, :], in0=xacc[:, ti, :], scalar=rstd, in1=bc2, op0=ALU.mult, op1=ALU.mult),
                     reads=[g, t_bc2], writes=[t_xa[ti]])
                P.op("sp", lambda e, ti=ti, r0=r0: e.dma_start(out=y_d[r0 + ti * 128:r0 + (ti + 1) * 128, :],
                                                               in_=xacc[:, ti, :]),
                     reads=[t_xa[ti]], dma="out%d" % ti)

        assert wq["acq"] == len(specs) and wq["released"] == len(specs), (wq, len(specs))
        P.emit(nc, sems, dsem, ["out%d" % i for i in range(4)])
    return nc


_NC_CACHE = {}


def _host_consts(core, c, b_ada, norm1_g, sinks, pool_scale, norm2_g, w_router_group, b_router_group,
                 w_router_expert, b_router_expert, final_g):
    b, q = core // 4, core % 4
    fm = lambda v: np.ascontiguousarray(v.reshape(-1, 128).T)
    cst = np.zeros((128, C_TOT), np.float32)
    cst[:, C_C:C_C + 16] = fm(c[b])
    cst[:, C_BADA:C_BADA + 96] = fm(b_ada[0])
    cst[:, C_G1:C_G1 + 16] = fm(norm1_g[0])
    cst[:, C_SINK:C_SINK + 16] = sinks[0][None, :]
    cst[:, C_PSC:C_PSC + 8] = fm(pool_scale[0])
    cst[:, C_G2:C_G2 + 16] = fm(norm2_g[0])
    cst[:, C_BR:C_BR + 4] = b_router_group[0][None, :]
    cst[:, C_BR + 4:C_BR + 20] = b_router_expert[0][None, :]
    cst[:, C_FG:C_FG + 16] = fm(final_g)
    cst[:, C_UF] = 0.0 if q == 0 else 1.0
    for g, w in enumerate((2, 4, 8, 16)):
        t = np.arange(16)
        cst[:, C_INVC + g * 16:C_INVC + (g + 1) * 16] = (1.0 / np.minimum(t + 1, w) if q == 0
                                                         else np.full(16, 1.0 / w))[None, :]
    r = np.arange(128)[:, None]
    j = np.arange(256)[None, :]
    valid = (j > r) & (j <= r + 128)
    am = np.where(valid, 0.0, NEG).astype(np.float32)
    cst[:, C_AM:C_AM + 256] = am
    am0 = am.copy()
    if q == 0:
        am0[:, :128] = NEG
    cst[:, C_AM0:C_AM0 + 256] = am0
    wr = np.concatenate([w_router_group[0], w_router_expert[0]], axis=1)
    cst[:, C_WR:C_WR + 320] = wr.reshape(16, 128, 20).transpose(1, 0, 2).reshape(128, 320)
    return cst


def kernel(x, c, w_ada, b_ada, norm1_g, w_in, sinks, w_pool, pool_scale, w_attn_branch, w_pool_branch, w_out,
           norm2_g, w_router_group, b_router_group, w_router_expert, b_router_expert, w_e_gate, w_e_up,
           w_e_down, final_g):
    f = lambda a: np.ascontiguousarray(np.asarray(a, dtype=np.float32))
    x = f(x)
    if "nc" not in _NC_CACHE:
        _NC_CACHE["nc"] = build_nc()
    nc = _NC_CACHE["nc"]
    shared = {
        "w_ada": f(w_ada)[0], "w_in": f(w_in)[0], "w_pool": f(w_pool)[0], "w_ab": f(w_attn_branch)[0],
        "w_pb": f(w_pool_branch)[0], "w_out": f(w_out)[0], "w_eg": f(w_e_gate)[0], "w_eu": f(w_e_up)[0],
        "w_ed": f(w_e_down)[0],
    }
    small = [np.asarray(a, dtype=np.float32) for a in (c, b_ada, norm1_g, sinks, pool_scale, norm2_g,
                                                        w_router_group, b_router_group, w_router_expert,
                                                        b_router_expert, final_g)]
    fg_bc = np.ascontiguousarray(np.broadcast_to(np.asarray(final_g, dtype=np.float32)[None, :], (128, D)))
    in_maps = []
    for core in range(NCORE):
        b, q = core // 4, core % 4
        xc = np.zeros((NTOK + 128, D), np.float32)
        if q > 0:
            xc[:128] = x[b, q * NTOK - 128:q * NTOK]
        xc[128:] = x[b, q * NTOK:(q + 1) * NTOK]
        m = dict(shared)
        m["x"] = xc
        m["cst"] = _host_consts(core, *small)
        m["fg_bc"] = fg_bc
        in_maps.append(m)
    res = run_bass_kernel_spmd(nc, in_maps, core_ids=list(range(NCORE)))
    out = np.empty((2, 4 * NTOK, D), np.float32)
    for core in range(NCORE):
        b, q = core // 4, core % 4
        out[b, q * NTOK:(q + 1) * NTOK] = res.results[core]["y"]
    return out
```

```python
import numpy as np
from contextlib import ExitStack
import concourse.bass as bass
import concourse.mybir as mybir
from concourse.bass_utils import run_bass_kernel_spmd

F32 = mybir.dt.float32
BF16 = mybir.dt.bfloat16
U8 = mybir.dt.uint8
AF = mybir.ActivationFunctionType
ALU = mybir.AluOpType
AX = mybir.AxisListType

D = 2048
NCORE = 8
NTOK = 2048
TB = 512
NB = NTOK // TB
HT = TB + 128
NEXP = 16
EPS = 1e-6
NEG = -1e30

C_C, C_BADA, C_G1, C_SINK, C_PSC, C_G2, C_BR, C_FG, C_UF, C_INVC, C_AM, C_AM0, C_WR = (
    0, 16, 112, 128, 144, 152, 168, 188, 204, 205, 269, 525, 781)
C_TOT = 781 + 320


class T:
    __slots__ = ("w", "r")

    def __init__(self):
        self.w = None
        self.r = {}


class Prog:
    ENGS = ("pe", "act", "dve", "pool", "sp")

    def __init__(self):
        self.ops = {e: [] for e in self.ENGS}
        self.seen = {e: {} for e in self.ENGS}
        self.dma_cnt = {}
        self.need = {e: set() for e in self.ENGS}

    def op(self, eng, fn, reads=(), writes=(), dma=None, same_ok=False):
        deps = {}

        def add(k, v):
            if deps.get(k, -1) < v:
                deps[k] = v

        for t in reads:
            if t.w is not None:
                add(t.w[:2], t.w[2])
        for t in writes:
            if t.w is not None:
                add(t.w[:2], t.w[2])
            for k, v in t.r.items():
                add(k, v)
        idx = len(self.ops[eng])
        waits = []
        sn = self.seen[eng]
        for k, val in deps.items():
            if k[0] == "e" and k[1] == eng and (eng == "pe" or same_ok):
                continue
            if sn.get(k, -1) >= val:
                continue
            sn[k] = val
            waits.append((k[0], k[1], val))
            if k[0] == "e":
                self.need[k[1]].add(val)
        if dma is not None:
            self.dma_cnt[dma] = self.dma_cnt.get(dma, 0) + 16
            me = ("d", dma, self.dma_cnt[dma])
        else:
            me = ("e", eng, idx)
        self.ops[eng].append((waits, fn, dma))
        for t in reads:
            if t.r.get(me[:2], -1) < me[2]:
                t.r[me[:2]] = me[2]
        for t in writes:
            t.w = me
            t.r = {}
        return me

    def fence(self, new, old):
        deps = {}
        for t in old:
            if t.w is not None and deps.get(t.w[:2], -1) < t.w[2]:
                deps[t.w[:2]] = t.w[2]
            for k, v in t.r.items():
                if deps.get(k, -1) < v:
                    deps[k] = v
        for t in new:
            t.w = None
            t.r = dict(deps)

    def emit(self, nc, sems, dma_sems, final_waits):
        rank = {}
        for e in self.ENGS:
            s = sorted(self.need[e])
            rank[e] = {v: i + 1 for i, v in enumerate(s)}
        handles = {"pe": "tensor", "act": "scalar", "dve": "vector", "pool": "gpsimd", "sp": "sync"}

        def replay(engname, e):
            rk = rank[engname]
            for idx, (waits, fn, dma) in enumerate(self.ops[engname]):
                for kind, key, val in waits:
                    if kind == "e":
                        e.wait_ge(sems[key], rank[key][val])
                    else:
                        e.wait_ge(dma_sems[key], val)
                ins = fn(e)
                if dma is not None:
                    ins.then_inc(dma_sems[dma], 16)
                elif idx in rk:
                    ins.then_inc(sems[engname], 1)
            if engname == "sp":
                for key in final_waits:
                    e.wait_ge(dma_sems[key], self.dma_cnt[key])

        with nc.Block() as block:
            for engname in self.ENGS:
                getattr(block, handles[engname])(lambda e, n=engname: replay(n, e))


ADA_PRO = 16
HOIST_AT = 6


def build_nc():
    nc = bass.Bass("TRN2", target_bir_lowering=False)

    def din(name, shape):
        return nc.dram_tensor(name, shape, F32, kind="ExternalInput").ap()

    x_d = din("x", [NTOK + 128, D])
    cst_d = din("cst", [128, C_TOT])
    w_ada = din("w_ada", [D, 6 * D])
    w_in = din("w_in", [D, 6656])
    w_pool = din("w_pool", [4, 256, 256])
    w_ab = din("w_ab", [1024, D])
    w_pb = din("w_pb", [1024, D])
    w_out = din("w_out", [D, D])
    w_eg = din("w_eg", [NEXP, D, 512])
    w_eu = din("w_eu", [NEXP, D, 512])
    w_ed = din("w_ed", [NEXP, 512, D])
    fg_d = din("fg_bc", [128, D])
    y_d = nc.dram_tensor("y", [NTOK, D], F32, kind="ExternalOutput").ap()

    P = Prog()
    with ExitStack() as st:
        def sb(name, shape, dt):
            return st.enter_context(nc.sbuf_tensor(name, shape, dt))

        NSLOT = 6
        SLOTE = 4096
        wring = [sb("wring%d" % i, [128, SLOTE], BF16) for i in range(NSLOT)]
        xacc = sb("xacc", [128, 4, D], F32)
        bc = sb("bc", [128, D], F32)
        cst = sb("cst_sb", [128, C_TOT], F32)
        ident_f = sb("ident_f", [128, 128], F32)
        ones_f = sb("ones_f", [128, 128], F32)
        ident_b = sb("ident_b", [128, 128], BF16)
        amask_b = sb("amask_b", [128, 512], BF16)
        ada_fm = sb("ada_fm", [128, 96], F32)
        a12 = sb("a12", [128, 32], F32)
        c_bf = sb("c_bf", [128, 16], BF16)
        stt_ = sb("stats", [128, 64], F32)
        stA = [sb("stA%d" % i, [128, 32], F32) for i in range(2)]
        dg = [sb("dg%d" % i, [128, 128], F32) for i in range(4)]
        dgrs = [sb("dgr%d" % i, [128, 128], F32) for i in range(2)]
        comb = sb("comb", [128, 4, 16], F32)
        rts = [sb("rt%d" % i, [128, 96], F32) for i in range(2)]
        OFFA, OFFB, OFFC = 0, 20480, 36864
        PPB = OFFC + 69760
        pp = sb("pp", [128, PPB], U8)
        ps = st.enter_context(nc.psum_tensor("ps", [128, 4096], F32))

        def view(off, n, dt, pat=None, **kw):
            sz = 2 if dt == BF16 else 4
            v = pp[:, off:off + n * sz].bitcast(dt)
            if pat:
                v = v.rearrange(pat, **kw)
            return v

        K3 = "p (k t) -> p k t"
        hT = view(OFFA, 16 * HT, BF16, K3, k=16)
        attnT = view(OFFB, 8 * TB, BF16, K3, k=8)
        pmixT = view(OFFB + 8192, 8 * TB, BF16, K3, k=8)
        actb = [view(OFFB + i * 4096, 4 * TB, BF16, K3, k=4) for i in range(2)]
        stmp = [view(OFFB + 8192 + i * 2048, 512, F32) for i in range(4)]
        xnj = [view(OFFC + i * 4096, D, BF16) for i in range(2)]
        xt = [view(OFFC + 8192 + i * 8192, D, F32) for i in range(2)]
        qT = view(OFFC + 24576, 8 * TB, BF16, K3, k=8)
        kT2 = view(OFFC + 32768, 4 * HT, BF16, K3, k=4)
        Vt = view(OFFC + 37888, 5 * 512, BF16, "p (t g d) -> p t g d", t=5, g=4)
        ub = [view(OFFC + 43008 + i * 2176, 544, F32) for i in range(2)]
        sAB = [view(OFFC + 47360 + i * 2176, 544, F32) for i in range(2)]
        pooled = view(OFFC + 51712, 8 * TB, BF16, K3, k=8)
        Pb = [view(OFFC + 59904 + i * 2048, 4 * 256, BF16, K3, k=4) for i in range(2)]
        PTs = [view(OFFC + 64000 + i * 2048, 1024, BF16) for i in range(2)]
        siga = view(OFFC, 16 * TB, BF16, K3, k=16)
        sigp_lo = view(OFFC + 16384, 8 * TB, BF16, K3, k=8)
        sigp_hi = view(OFFC + 43008, 8 * TB, BF16, K3, k=8)
        mergedT = view(OFFC + 24576, 16 * TB, BF16, K3, k=16)
        tA = [view(OFFC + 59904 + i * 2048, 512, F32) for i in range(2)]
        tB = [view(OFFC + 64000 + i * 2048, 512, F32) for i in range(2)]
        xn2j = [view(OFFC + i * 4096, D, BF16) for i in range(2)]
        h2Tf = [view(OFFC + 8192 + i * 8192, 16 * 128, F32, K3, k=16) for i in range(2)]
        sl = [view(OFFC + 24576 + i * 2048, 512, F32) for i in range(2)]
        h2T = view(OFFC + 28672, 16 * TB, BF16, K3, k=16)
        fjunk = view(OFFC + 45056, D, BF16)
        bc2 = view(OFFC + 49152, D, F32)

        sems = {e: st.enter_context(nc.semaphore("s_" + e)) for e in Prog.ENGS}
        dnames = ["w%d" % i for i in range(NSLOT)] + ["xt0", "xt1", "cst", "kdup", "bc2", "bcs", "bcl"] + \
                 ["xa%d" % i for i in range(4)] + ["out%d" % i for i in range(4)]
        dsem = {k: st.enter_context(nc.semaphore("d_" + k)) for k in dnames}

        t_w = [T() for _ in range(NSLOT)]
        t_pb = [T() for _ in range(8)]
        t_cst, t_id, t_ada, t_a12, t_cbf, t_bc, t_comb, t_ada2, t_a2, t_ada2a = [T() for _ in range(10)]
        t_dg = [T() for _ in range(4)]
        t_xa = [T() for _ in range(4)]
        t_hT = [T() for _ in range(5)]
        t_h2T = [T() for _ in range(4)]
        t_attn = [T() for _ in range(4)]
        t_pmix = T()
        t_act = [T(), T()]
        t_stmp = [T() for _ in range(4)]
        t_xnj, t_xt = [T(), T()], [T(), T()]
        t_dgr = [T(), T()]
        t_q, t_kn, t_kd, t_v = T(), T(), T(), T()
        t_ub, t_s = [T(), T()], [T(), T()]
        t_pooled = [T() for _ in range(8)]
        t_P, t_PT = [T(), T()], [T(), T()]
        t_sa = [T(), T()]
        t_merged = [T() for _ in range(4)]
        t_siga, t_sigp = [T() for _ in range(4)], [T() for _ in range(4)]
        t_tA, t_tB = [T(), T()], [T(), T()]
        t_xn2j, t_h2Tf, t_sl = [T(), T()], [T(), T()], [T(), T()]
        t_st = [T() for _ in range(8)]
        t_rts = [T(), T()]
        regA_M = t_hT
        regA_E = []
        regB_M = t_attn + [t_pmix]
        regB_E = t_act + t_stmp
        regC_M1_rest = [t_q, t_kn, t_kd, t_v] + t_ub + t_s + t_pooled + t_P + t_PT
        regC_M1 = t_xnj + t_xt + regC_M1_rest
        regC_M2 = t_merged + t_siga + t_sigp + t_tA + t_tB
        t_fj, t_bc2 = T(), T()
        t_gsc = [T(), T()]
        regC_E = t_xn2j + t_h2Tf + t_sl + t_h2T + [t_fj, t_bc2]

        state = {"bank": 0, "ev": 0, "resv": set(), "live": set()}

        def bank(n=1):
            b0 = state["bank"]
            for off in range(0, 16):
                b = (b0 + off) % 8
                if n > 1 and b % n:
                    continue
                if b + n > 8:
                    continue
                if any((b + i) in state["resv"] or (b + i) in state["live"] for i in range(n)):
                    continue
                for i in range(n):
                    state["live"].add(b + i)
                state["bank"] = (b + n) % 8
                return b
            raise AssertionError("out of PSUM banks: live=%s resv=%s" % (state["live"], state["resv"]))

        def bfree(b, n=1):
            for i in range(n):
                state["live"].discard(b + i)

        def pbank(b, n=1, c0=0, c1=None):
            if c1 is None:
                c1 = 512 * n
            return ps[:, b * 512 + c0:b * 512 + c1]

        def cs(c0, n):
            return cst[:, c0:c0 + n]

        def kcols(w2d, c0, ncols, k0, nk):
            src = w2d.rearrange("(k p) c -> p k c", p=128)[:, k0:k0 + nk, c0:c0 + ncols]
            return [(lambda sl_: sl_[:, 0:nk * ncols].rearrange(K3, k=nk), src)]

        def all_loads():
            for i in range(ADA_PRO):
                yield ("ada", kcols(w_ada, i * 256, 256, 0, 16))
            ada_i = [ADA_PRO]

            def ada_more(n=1):
                out = []
                for _ in range(n):
                    if ada_i[0] < 48:
                        out.append(("ada", kcols(w_ada, ada_i[0] * 256, 256, 0, 16)))
                        ada_i[0] += 1
                return out

            for blk in range(NB):
                for i in range(4):
                    yield ("q", kcols(w_in, i * 256, 256, 0, 16))
                yield ("k", kcols(w_in, 1024, 256, 0, 16))
                yield ("v", kcols(w_in, 1280, 256, 0, 16))
                for i in range(4):
                    yield ("u", kcols(w_in, 1536 + i * 256, 256, 0, 16))
                for gi in range(16):
                    yield ("g", kcols(w_in, 2560 + (gi // 8) * 2048 + (gi % 8) * 256, 256, 0, 16))
                    yield from ada_more()
                yield ("wp", [(lambda sl_: sl_[:, 0:2048].rearrange("p (g k d) -> p g k d", g=4, k=2),
                               w_pool.rearrange("g (k p) d -> p g k d", p=128))])
                yield from ada_more()
                for og in range(4):
                    yield ("brA", kcols(w_ab, og * 512, 512, 0, 8))
                    yield ("brB", kcols(w_pb, og * 512, 512, 0, 8))
                    yield from ada_more(2)
                for db in range(4):
                    for kh in range(2):
                        yield ("wo", kcols(w_out, db * 512, 512, kh * 8, 8))
                        yield from ada_more()
                for ex in range(NEXP):
                    for hh in range(2):
                        yield ("eg", kcols(w_eg[ex], hh * 256, 256, 0, 16))
                        yield ("eu", kcols(w_eu[ex], hh * 256, 256, 0, 16))
                    for dh in range(2):
                        yield ("ed", kcols(w_ed[ex], dh * 1024, 1024, 0, 4))

        specs = list(all_loads())
        wq = {"issued": 0, "released": 0, "acq": 0}

        def pump():
            while wq["issued"] < len(specs) and wq["issued"] < wq["released"] + NSLOT:
                i = wq["issued"]
                s = i % NSLOT
                for dfn, src in specs[i][1]:
                    d = dfn(wring[s])
                    P.op("pool", lambda e, d=d, src=src: e.dma_start(out=d, in_=src), writes=[t_w[s]],
                         dma="w%d" % s)
                wq["issued"] += 1

        def wnext(tag):
            i = wq["acq"]
            assert specs[i][0] == tag, (specs[i][0], tag, i)
            pump()
            assert wq["issued"] > i, "weight queue: too many held"
            wq["acq"] += 1
            return i % NSLOT

        def wrel(n=1):
            wq["released"] += n
            pump()

        def slot3(s, k):
            return wring[s][:, :].rearrange(K3, k=k)

        def evac(out_ap, in_ap, reads, writes, scale=None, same_ok=True):
            state["ev"] ^= 1
            if state["ev"]:
                if scale is None:
                    P.op("act", lambda e: e.activation(out=out_ap, in_=in_ap, func=AF.Copy),
                         reads=reads, writes=writes, same_ok=same_ok)
                else:
                    P.op("act", lambda e: e.activation(out=out_ap, in_=in_ap, func=AF.Copy, scale=scale),
                         reads=reads, writes=writes, same_ok=same_ok)
            else:
                if scale is None:
                    P.op("dve", lambda e: e.tensor_copy(out=out_ap, in_=in_ap),
                         reads=reads, writes=writes, same_ok=same_ok)
                else:
                    P.op("dve", lambda e: e.tensor_scalar(out=out_ap, in0=in_ap, scalar1=scale, scalar2=None,
                                                         op0=ALU.mult),
                         reads=reads, writes=writes, same_ok=same_ok)

        def mm(out_ap, pairs, reads, btiles, start=True, stop=True):
            n = len(pairs)
            for i, (l, r) in enumerate(pairs):
                P.op("pe", lambda e, l=l, r=r, i=i: e.matmul(out_ap, lhsT=l, rhs=r, start=(start and i == 0),
                                                            stop=(stop and i == n - 1)),
                     reads=reads, writes=btiles)

        P.op("sp", lambda e: e.dma_start(out=cst[:, :], in_=cst_d), writes=[t_cst], dma="cst")
        pump()
        P.op("pool", lambda e: e.memset(ident_f[:, :], 0.0), writes=[t_id])
        P.op("pool", lambda e: e.affine_select(out=ident_f[:, :], in_=ident_f[:, :], pattern=[[-1, 128]],
                                              compare_op=ALU.not_equal, fill=1.0, base=0,
                                              channel_multiplier=1), reads=[t_id], writes=[t_id])
        P.op("pool", lambda e: e.memset(ones_f[:, :], 1.0), writes=[t_id], same_ok=True)
        cs_eps = stt_[:, 28:29]
        P.op("pool", lambda e: e.memset(stt_[:, 28:29], EPS), writes=[t_id], same_ok=True)
        P.op("dve", lambda e: e.tensor_copy(out=c_bf[:, :], in_=cs(C_C, 16)), reads=[t_cst], writes=[t_cbf])
        P.op("dve", lambda e: e.tensor_copy(out=ident_b[:, :], in_=ident_f[:, :]), reads=[t_id], writes=[t_id])
        P.op("dve", lambda e: e.tensor_copy(out=amask_b[:, :], in_=cs(C_AM, 512)), reads=[t_cst, t_id],
             writes=[t_id])

        bA = bank()
        bfree(bA)
        state["resv"].add(bA)
        ada_n = [0]

        def ada_consume(n=1):
            for _ in range(n):
                if ada_n[0] >= 48:
                    return
                s_i = ada_n[0]
                ada_n[0] += 1
                s = wnext("ada")
                w3 = slot3(s, 16)
                for nn in range(2):
                    j = s_i * 2 + nn
                    mm(ps[:, bA * 512 + j:bA * 512 + j + 1],
                       [(w3[:, k, nn * 128:(nn + 1) * 128], c_bf[:, k:k + 1]) for k in range(16)],
                       [t_w[s], t_cbf], [t_pb[bA]])
                wrel()

        ada_consume(ADA_PRO)
        P.op("dve", lambda e: e.tensor_tensor(out=ada_fm[:, 0:32], in0=ps[:, bA * 512:bA * 512 + 32],
                                             in1=cs(C_BADA, 32), op=ALU.add),
             reads=[t_pb[bA], t_cst], writes=[t_ada])
        P.op("dve", lambda e: e.scalar_tensor_tensor(out=a12[:, 0:16], in0=ada_fm[:, 16:32], scalar=1.0,
                                                    in1=cs(C_G1, 16), op0=ALU.add, op1=ALU.mult),
             reads=[t_ada, t_cst], writes=[t_a12])

        def ada_finish_a():
            assert ada_n[0] >= 24
            P.op("dve", lambda e: e.tensor_tensor(out=ada_fm[:, 32:48], in0=ps[:, bA * 512 + 32:bA * 512 + 48],
                                                 in1=cs(C_BADA + 32, 16), op=ALU.add),
                 reads=[t_pb[bA], t_cst], writes=[t_ada2a])

        def ada_finish():
            assert ada_n[0] == 48
            P.op("dve", lambda e: e.tensor_tensor(out=ada_fm[:, 48:96], in0=ps[:, bA * 512 + 48:bA * 512 + 96],
                                                 in1=cs(C_BADA + 48, 48), op=ALU.add),
                 reads=[t_pb[bA], t_cst], writes=[t_ada2])
            P.op("dve", lambda e: e.scalar_tensor_tensor(out=a12[:, 16:32], in0=ada_fm[:, 64:80], scalar=1.0,
                                                        in1=cs(C_G2, 16), op0=ALU.add, op1=ALU.mult),
                 reads=[t_ada2, t_cst], writes=[t_a2])
            state["resv"].discard(bA)

        def regen_bc(vec_ap, vec_tiles, factor=1.0, dst=None, tdst=None):
            dst = bc if dst is None else dst
            tdst = t_bc if tdst is None else tdst
            for j4 in range(4):
                b = bank()
                for jj in range(4):
                    j = j4 * 4 + jj
                    P.op("dve", lambda e, j=j: e.tensor_scalar(out=dg[j % 4][:, :], in0=ident_f[:, :],
                                                              scalar1=vec_ap[:, j:j + 1], scalar2=factor,
                                                              op0=ALU.mult, op1=ALU.mult),
                         reads=[t_id] + vec_tiles, writes=[t_dg[j % 4]])
                    P.op("pe", lambda e, j=j, jj=jj, b=b: e.matmul(pbank(b, 1, jj * 128, jj * 128 + 128),
                                                                  lhsT=ones_f[:, :], rhs=dg[j % 4][:, :],
                                                                  start=True, stop=True),
                         reads=[t_id, t_dg[j % 4]], writes=[t_pb[b]])
                evac(dst[:, j4 * 512:(j4 + 1) * 512], pbank(b), [t_pb[b]], [tdst])
                bfree(b)

        def rms_stats(src_ap, src_tiles, junk_ap, junk_tiles, slot_i):
            g = t_st[slot_i]
            c = 32 + slot_i * 4
            P.op("act", lambda e: e.activation(out=junk_ap, in_=src_ap, func=AF.Square,
                                              accum_out=stt_[:, c:c + 1]),
                 reads=src_tiles, writes=junk_tiles + [g])
            P.op("act", lambda e: e.activation(out=stt_[:, c + 1:c + 2], in_=stt_[:, c:c + 1], func=AF.Sqrt,
                                              scale=1.0 / D, bias=cs_eps),
                 reads=[g, t_id], writes=[g])
            P.op("dve", lambda e: e.reciprocal(out=stt_[:, c + 2:c + 3], in_=stt_[:, c + 1:c + 2]),
                 reads=[g], writes=[g])
            return stt_[:, c + 2:c + 3], g

        def emit_M1a(r0):
            m1 = {}

            def m1_N1(ti):
                xb = xt[ti % 2]
                P.op("sp", lambda e, xb=xb, ti=ti, r0=r0: e.dma_start(
                    out=xb, in_=x_d[r0 + ti * 128:r0 + (ti + 1) * 128, :]),
                     writes=[t_xt[ti % 2]], dma="xt%d" % (ti % 2))
                rstd, g = rms_stats(xb, [t_xt[ti % 2]], xnj[ti % 2], [t_xnj[ti % 2]], ti % 2)
                dgr, tdgr = dgrs[ti % 2], t_dgr[ti % 2]
                P.op("dve", lambda e: e.tensor_scalar(out=dgr[:, :], in0=ident_f[:, :], scalar1=rstd,
                                                     scalar2=None, op0=ALU.mult),
                     reads=[t_id, g], writes=[tdgr])

            def m1_N2(ti):
                xb = xt[ti % 2]
                dgr, tdgr = dgrs[ti % 2], t_dgr[ti % 2]
                b4 = bank(4)
                m1[ti] = b4
                for c in range(16):
                    P.op("pe", lambda e, c=c: e.matmul(
                        ps[:, b4 * 512 + c * 128:b4 * 512 + (c + 1) * 128], lhsT=xb[:, c * 128:(c + 1) * 128],
                        rhs=dgr[:, :], start=True, stop=True),
                         reads=[t_xt[ti % 2], tdgr], writes=[t_pb[b4 + c // 4]])

            def m1_N3(ti):
                b4 = m1.pop(ti)
                for c in range(16):
                    src = ps[:, b4 * 512 + c * 128:b4 * 512 + (c + 1) * 128]
                    dst = hT[:, c, ti * 128:(ti + 1) * 128]
                    if c % 2 == 0:
                        P.op("act", lambda e, c=c, src=src, dst=dst: e.activation(
                            out=dst, in_=src, func=AF.Identity, scale=a12[:, c:c + 1], bias=ada_fm[:, c:c + 1]),
                             reads=[t_pb[b4 + c // 4], t_a12, t_ada], writes=[t_hT[ti]], same_ok=True)
                    else:
                        P.op("dve", lambda e, c=c, src=src, dst=dst: e.tensor_scalar(
                            out=dst, in0=src, scalar1=a12[:, c:c + 1], scalar2=ada_fm[:, c:c + 1],
                            op0=ALU.mult, op1=ALU.add),
                             reads=[t_pb[b4 + c // 4], t_a12, t_ada], writes=[t_hT[ti]], same_ok=True)
                bfree(b4, 4)

            def stage(k):
                if k == 0:
                    m1_N1(0)
                    m1_N1(1)
                else:
                    ti = k - 1
                    m1_N2(ti)
                    m1_N3(ti)
                    if ti + 2 < 5:
                        m1_N1(ti + 2)
            return stage


        for blk in range(NB):
            r0 = blk * TB
            if blk > 0:
                P.fence(regB_M, regB_E)
                P.fence(regC_M1_rest, regC_E + regC_M2)
            if blk == 0:
                st0 = emit_M1a(0)
                for k in range(6):
                    st0(k)

            hmain = lambda k: hT[:, k, 128:HT]
            for s_i in range(4):
                s = wnext("q")
                w3 = slot3(s, 16)
                for cc in range(2):
                    ch = s_i * 2 + cc
                    b = bank()
                    mm(pbank(b), [(w3[:, k, cc * 128:(cc + 1) * 128], hmain(k)) for k in range(16)],
                       [t_w[s]] + t_hT, [t_pb[b]])
                    evac(qT[:, ch, :], pbank(b), [t_pb[b]], [t_q], scale=0.125)
                    bfree(b)
                wrel()
            s = wnext("k")
            w3 = slot3(s, 16)
            for ch in range(2):
                b = bank()
                mm(pbank(b), [(w3[:, k, ch * 128:(ch + 1) * 128], hmain(k)) for k in range(16)],
                   [t_w[s]] + t_hT, [t_pb[b]])
                b_h = bank()
                mm(pbank(b_h, 1, 0, 128), [(w3[:, k, ch * 128:(ch + 1) * 128], hT[:, k, 0:128]) for k in range(16)],
                   [t_w[s]] + t_hT, [t_pb[b_h]])
                for hf_ in range(2):
                    kv = ch * 2 + hf_
                    o = hf_ * 64
                    evac(kT2[o:o + 64, kv, 128:HT], ps[o:o + 64, b * 512:b * 512 + 512], [t_pb[b]], [t_kn])
                    evac(kT2[o:o + 64, kv, 0:128], ps[o:o + 64, b_h * 512:b_h * 512 + 128], [t_pb[b_h]], [t_kn])
                bfree(b)
                bfree(b_h)
            wrel()
            for kv in range(4):
                o = (kv % 2) * 64
                P.op("sp", lambda e, kv=kv, o=o: e.dma_start(out=kT2[64 - o:128 - o, kv, :], in_=kT2[o:o + 64, kv, :]),
                     reads=[t_kn], writes=[t_kd], dma="kdup")
            s = wnext("v")
            w3 = slot3(s, 16)
            for ti in range(5):
                b = bank()
                mm(pbank(b, 1, 0, 256), [(hT[:, k, ti * 128:(ti + 1) * 128], w3[:, k, :]) for k in range(16)],
                   [t_w[s]] + t_hT, [t_pb[b]])
                src = pbank(b, 1, 0, 256).rearrange("p (g d) -> p g d", g=4)
                evac(Vt[:, ti, :, 0:64], src, [t_pb[b]], [t_v])
                evac(Vt[:, ti, :, 64:128], src, [t_pb[b]], [t_v])
                bfree(b)
            wrel()

            att_state = {}

            def att_A(i):
                qt, kv = divmod(i, 4)
                mcol = 256 if (blk == 0 and qt == 0) else 0
                b2 = bank(2)
                for j in range(4):
                    h = kv * 4 + j
                    off = (h % 2) * 64
                    o_ap = ps[:, b2 * 512 + j * 256:b2 * 512 + (j + 1) * 256]
                    P.op("pe", lambda e, o_ap=o_ap, off=off, h=h, qt=qt, kv=kv: e.matmul(
                        o_ap, lhsT=qT[off:off + 64, h // 2, qt * 128:(qt + 1) * 128],
                        rhs=kT2[off:off + 64, kv, qt * 128:(qt + 2) * 128], start=True, stop=False),
                         reads=[t_q, t_kn, t_kd], writes=[t_pb[b2 + j // 2]])
                    P.op("pe", lambda e, o_ap=o_ap, mcol=mcol: e.matmul(
                        o_ap, lhsT=ident_b[:, :], rhs=amask_b[:, mcol:mcol + 256], start=False, stop=True),
                         reads=[t_id], writes=[t_pb[b2 + j // 2]])
                att_state[i] = b2

            def att_ctx(i):
                qt, kv = divmod(i, 4)
                return dict(qt=qt, kv=kv, S=stA[i % 2], tS=t_sa[i % 2], Pn=Pb[i % 2], tP=t_P[i % 2],
                            PTb=PTs[i % 2], tPT=t_PT[i % 2], sink4=cs(C_SINK + kv * 4, 4))

            def att_B1(i):
                c = att_ctx(i)
                S, tS, sink4 = c["S"], c["tS"], c["sink4"]
                b2 = att_state[i]
                pbs = [t_pb[b2], t_pb[b2 + 1]]
                P.op("dve", lambda e: e.tensor_reduce(out=S[:, 0:4], in_=pbank(b2, 2).rearrange(K3, k=4), axis=AX.X,
                                                     op=ALU.max), reads=pbs, writes=[tS])
                P.op("dve", lambda e: e.tensor_tensor(out=S[:, 0:4], in0=S[:, 0:4], in1=sink4, op=ALU.max),
                     reads=[tS, t_cst], writes=[tS])
                P.op("dve", lambda e: e.tensor_scalar(out=S[:, 4:8], in0=S[:, 0:4], scalar1=-1.0, scalar2=None,
                                                     op0=ALU.mult), reads=[tS], writes=[tS])
                P.op("dve", lambda e: e.tensor_tensor(out=S[:, 8:12], in0=sink4, in1=S[:, 0:4], op=ALU.subtract),
                     reads=[tS, t_cst], writes=[tS], same_ok=True)

            def att_B2(i):
                c = att_ctx(i)
                S, tS, Pn, tP = c["S"], c["tS"], c["Pn"], c["tP"]
                b2 = att_state.pop(i)
                pbs = [t_pb[b2], t_pb[b2 + 1]]
                for j in range(4):
                    P.op("act", lambda e, j=j: e.activation(
                        out=Pn[:, j, :], in_=ps[:, b2 * 512 + j * 256:b2 * 512 + (j + 1) * 256], func=AF.Exp,
                        bias=S[:, 4 + j:5 + j], scale=1.0, accum_out=S[:, 12 + j:13 + j]),
                         reads=pbs + [tS], writes=[tP, tS], same_ok=True)
                P.op("act", lambda e: e.activation(out=S[:, 16:20], in_=S[:, 8:12], func=AF.Exp),
                     reads=[tS], writes=[tS], same_ok=True)
                bfree(b2, 2)

            def att_B3(i):
                c = att_ctx(i)
                S, tS, Pn, tP = c["S"], c["tS"], c["Pn"], c["tP"]
                P.op("dve", lambda e: e.tensor_tensor(out=S[:, 20:24], in0=S[:, 12:16], in1=S[:, 16:20], op=ALU.add),
                     reads=[tS], writes=[tS])
                P.op("dve", lambda e: e.reciprocal(out=S[:, 24:28], in_=S[:, 20:24]), reads=[tS], writes=[tS])
                P.op("dve", lambda e: e.tensor_tensor(
                    out=Pn[:, :, :], in0=Pn[:, :, :], in1=S[:, 24:28].unsqueeze(2).broadcast_to([128, 4, 256]),
                    op=ALU.mult), reads=[tP, tS], writes=[tP])

            def att_B4(i):
                c = att_ctx(i)
                Pn, tP, PTb, tPT = c["Pn"], c["tP"], c["PTb"], c["tPT"]
                bt = bank()
                ptv = pbank(bt).bitcast(BF16)
                for half in range(2):
                    for j in range(4):
                        o = (half * 4 + j) * 128
                        P.op("pe", lambda e, o=o, j=j, half=half: e.transpose(
                            out=ptv[:, o:o + 128], in_=Pn[:, j, half * 128:(half + 1) * 128],
                            identity=ident_b[:, :]), reads=[tP, t_id], writes=[t_pb[bt]])
                evac(PTb, ptv, [t_pb[bt]], [tPT])
                bfree(bt)

            def att_B5(i):
                c = att_ctx(i)
                qt, kv, PTb, tPT = c["qt"], c["kv"], c["PTb"], c["tPT"]
                bo = bank()
                mm(pbank(bo), [(Vt[:, qt, kv, :], PTb[:, 0:512]), (Vt[:, qt + 1, kv, :], PTb[:, 512:1024])],
                   [t_v, tPT], [t_pb[bo]])
                for j in range(4):
                    h = kv * 4 + j
                    o2 = (h % 2) * 64
                    srcp = ps[o2:o2 + 64, bo * 512 + j * 128:bo * 512 + (j + 1) * 128]
                    dstp = attnT[o2:o2 + 64, h // 2, qt * 128:(qt + 1) * 128]
                    if j % 2 == 0:
                        P.op("act", lambda e, srcp=srcp, dstp=dstp: e.activation(out=dstp, in_=srcp, func=AF.Copy),
                             reads=[t_pb[bo]], writes=[t_attn[qt]], same_ok=True)
                    else:
                        P.op("dve", lambda e, srcp=srcp, dstp=dstp: e.tensor_copy(out=dstp, in_=srcp),
                             reads=[t_pb[bo]], writes=[t_attn[qt]], same_ok=True)
                bfree(bo)

            def u_chunk(s, n_in, ch):
                w3 = slot3(s, 16)
                g = ch // 2
                wdw = 2 << g
                u = ub[ch % 2]
                tu = t_ub[ch % 2]
                b = bank()
                mm(pbank(b), [(w3[:, k, n_in * 128:(n_in + 1) * 128], hmain(k)) for k in range(16)],
                   [t_w[s]] + t_hT, [t_pb[b]])
                P.op("act", lambda e: e.activation(out=u[:, 16:528], in_=pbank(b), func=AF.Copy),
                     reads=[t_pb[b]], writes=[tu])
                bfree(b)
                b2 = bank()
                mm(pbank(b2, 1, 0, 16), [(w3[:, k, n_in * 128:(n_in + 1) * 128], hT[:, k, 112:128]) for k in range(16)],
                   [t_w[s]] + t_hT, [t_pb[b2]])
                if blk == 0:
                    P.op("dve", lambda e: e.tensor_scalar(out=u[:, 0:16], in0=pbank(b2, 1, 0, 16),
                                                         scalar1=cs(C_UF, 1), scalar2=None, op0=ALU.mult),
                         reads=[t_pb[b2], t_cst], writes=[tu], same_ok=True)
                    bfree(b2)
                else:
                    P.op("dve", lambda e: e.tensor_copy(out=u[:, 0:16], in_=pbank(b2, 1, 0, 16)),
                         reads=[t_pb[b2]], writes=[tu], same_ok=True)
                bfree(b2)
                cur, tcur = u, tu
                step = 1
                i = 0
                while step < wdw:
                    nxt, tn = sAB[i % 2], t_s[i % 2]
                    P.op("pool", lambda e, cur=cur, nxt=nxt, step=step: e.tensor_tensor(
                        out=nxt[:, step:528], in0=cur[:, step:528], in1=cur[:, 0:528 - step], op=ALU.add),
                         reads=[tcur], writes=[tn])
                    cur, tcur = nxt, tn
                    step *= 2
                    i += 1
                P.op("dve", lambda e, cur=cur: e.scalar_tensor_tensor(
                    out=pooled[:, ch, :], in0=cur[:, 16:528], scalar=1.0 / wdw, in1=u[:, 16:528],
                    op0=ALU.mult, op1=ALU.subtract),
                     reads=[tcur, tu], writes=[t_pooled[ch]])
                if blk == 0:
                    oth, toth = sAB[i % 2], t_s[i % 2]
                    P.op("dve", lambda e, cur=cur, oth=oth: e.tensor_tensor(
                        out=oth[:, 0:16], in0=cur[:, 16:32], in1=cs(C_INVC + g * 16, 16), op=ALU.mult),
                         reads=[tcur, t_cst], writes=[toth])
                    P.op("dve", lambda e, oth=oth: e.tensor_tensor(
                        out=pooled[:, ch, 0:16], in0=oth[:, 0:16], in1=u[:, 16:32], op=ALU.subtract),
                         reads=[toth, tu], writes=[t_pooled[ch]])

            u_slot = None
            for ch in range(8):
                if ch % 2 == 0:
                    u_slot = wnext("u")
                u_chunk(u_slot, ch % 2, ch)
                if ch % 2 == 1:
                    wrel()
            P.fence(t_siga + t_sigp[0:2], t_xnj + t_xt)
            P.fence(t_sigp[2:4], t_ub + t_s)

            def gate_unit(gi):
                which, hh = divmod(gi, 8)
                s = wnext("g")
                w3 = slot3(s, 16)
                for nn in range(2):
                    ch = hh * 2 + nn
                    b = bank()
                    mm(pbank(b), [(w3[:, k, nn * 128:(nn + 1) * 128], hmain(k)) for k in range(16)],
                       [t_w[s]] + t_hT, [t_pb[b]])
                    if which == 0:
                        dst, tdst = siga[:, ch, :], t_siga[ch // 4]
                    elif ch < 8:
                        dst, tdst = sigp_lo[:, ch, :], t_sigp[ch // 4]
                    else:
                        dst, tdst = sigp_hi[:, ch - 8, :], t_sigp[ch // 4]
                    P.op("act", lambda e, b=b, dst=dst: e.activation(out=dst, in_=pbank(b), func=AF.Tanh, scale=0.5),
                         reads=[t_pb[b]], writes=[tdst], same_ok=True)
                    bfree(b)
                wrel()
                ada_consume()

            for sst in range(16 + 3):
                if sst < 16:
                    att_A(sst)
                    gate_unit(sst)
                if sst == 10:
                    if blk == 0:
                        ada_finish_a()
                    regen_bc(ada_fm[:, 32:48], [t_ada2a], factor=0.5)
                if 0 <= sst - 2 < 16:
                    att_B3(sst - 2)
                if 0 <= sst - 1 < 16:
                    att_B1(sst - 1)
                    att_B2(sst - 1)
                if 0 <= sst - 2 < 16:
                    att_B4(sst - 2)
                if 0 <= sst - 3 < 16:
                    att_B5(sst - 3)

            s = wnext("wp")
            wp4 = wring[s][:, 0:2048].rearrange("p (g k d) -> p g k d", g=4, k=2)
            for g in range(4):
                for oc in range(2):
                    b = bank()
                    mm(pbank(b), [(wp4[:, g, kc, oc * 128:(oc + 1) * 128], pooled[:, g * 2 + kc, :]) for kc in range(2)],
                       [t_w[s]] + t_pooled, [t_pb[b]])
                    ch = g * 2 + oc
                    P.op("act", lambda e, b=b, ch=ch: e.activation(out=pmixT[:, ch, :], in_=pbank(b), func=AF.Identity,
                                                                  scale=cs(C_PSC + ch, 1), bias=0.0),
                         reads=[t_pb[b], t_cst], writes=[t_pmix], same_ok=True)
                    bfree(b)
            wrel()
            ada_consume()

            P.fence(t_merged, [t_q, t_kn, t_kd, t_v])
            P.fence(t_tA + t_tB, t_P + t_PT)
            for og in range(4):
                sA_ = wnext("brA")
                sB_ = wnext("brB")
                wa3, wb3 = slot3(sA_, 8), slot3(sB_, 8)
                for n in range(4):
                    ba = bank()
                    mm(pbank(ba), [(wa3[:, kc, n * 128:(n + 1) * 128], attnT[:, kc, :]) for kc in range(8)],
                       [t_w[sA_]] + t_attn, [t_pb[ba]])
                    bb = bank()
                    mm(pbank(bb), [(wb3[:, kc, n * 128:(n + 1) * 128], pmixT[:, kc, :]) for kc in range(8)],
                       [t_w[sB_], t_pmix], [t_pb[bb]])
                    i2 = n % 2
                    ch = og * 4 + n
                    ga_ap = siga[:, ch, :]
                    gp_ap = sigp_lo[:, ch, :] if ch < 8 else sigp_hi[:, ch - 8, :]
                    P.op("dve", lambda e, ba=ba, ga_ap=ga_ap, i2=i2: e.scalar_tensor_tensor(
                        out=tA[i2], in0=ga_ap, scalar=1.0, in1=pbank(ba), op0=ALU.add, op1=ALU.mult),
                         reads=[t_pb[ba], t_siga[og]], writes=[t_tA[i2]])
                    P.op("dve", lambda e, bb=bb, gp_ap=gp_ap, i2=i2: e.scalar_tensor_tensor(
                        out=tB[i2], in0=gp_ap, scalar=1.0, in1=pbank(bb), op0=ALU.add, op1=ALU.mult),
                         reads=[t_pb[bb], t_sigp[og]], writes=[t_tB[i2]])
                    bfree(ba)
                    bfree(bb)
                    P.op("pool", lambda e, og=og, n=n, i2=i2: e.tensor_tensor(out=mergedT[:, og * 4 + n, :], in0=tA[i2],
                                                                             in1=tB[i2], op=ALU.add),
                         reads=[t_tA[i2], t_tB[i2]], writes=[t_merged[og]], same_ok=True)
                wrel(2)
                ada_consume(2)

            for ti in range(4):
                P.op("sp", lambda e, ti=ti, r0=r0: e.dma_start(
                    out=xacc[:, ti, :], in_=x_d[r0 + 128 + ti * 128:r0 + 128 + (ti + 1) * 128, :]),
                     writes=[t_xa[ti]], dma="xa%d" % ti)
            k_ev = 0
            for db in range(4):
                bks = [bank() for _ in range(4)]
                for kh in range(2):
                    s = wnext("wo")
                    w3 = slot3(s, 8)
                    for ti in range(4):
                        mm(pbank(bks[ti]), [(mergedT[:, kh * 8 + k, ti * 128:(ti + 1) * 128], w3[:, k, :])
                                            for k in range(8)],
                           [t_w[s]] + t_merged, [t_pb[bks[ti]]], start=(kh == 0), stop=(kh == 1))
                    wrel()
                    ada_consume()
                for ti in range(4):
                    b = bks[ti]
                    i4 = k_ev % 2
                    k_ev += 1
                    P.op("dve", lambda e, b=b, db=db, i4=i4: e.tensor_tensor(out=tA[i4], in0=pbank(b),
                                                                            in1=bc[:, db * 512:(db + 1) * 512],
                                                                            op=ALU.mult),
                         reads=[t_pb[b], t_bc], writes=[t_tA[i4]])
                    bfree(b)
                    P.op("pool", lambda e, ti=ti, db=db, i4=i4: e.tensor_tensor(
                        out=xacc[:, ti, db * 512:(db + 1) * 512], in0=xacc[:, ti, db * 512:(db + 1) * 512],
                        in1=tA[i4], op=ALU.add),
                         reads=[t_tA[i4]], writes=[t_xa[ti]], same_ok=True)

            if blk == 0:
                ada_finish()
            P.fence(regB_E, regB_M)
            P.fence(regC_E, regC_M2)
            n2 = {}

            def n2_N1(ti):
                rstd, g = rms_stats(xacc[:, ti, :], [t_xa[ti]], xn2j[ti % 2], [t_xn2j[ti % 2]], 4 + ti % 2)
                dgr, tdgr = dgrs[ti % 2], t_dgr[ti % 2]
                P.op("dve", lambda e: e.tensor_scalar(out=dgr[:, :], in0=ident_f[:, :], scalar1=rstd,
                                                     scalar2=None, op0=ALU.mult),
                     reads=[t_id, g], writes=[tdgr])

            def n2_N2(ti):
                dgr, tdgr = dgrs[ti % 2], t_dgr[ti % 2]
                b4 = bank(4)
                n2[ti] = b4
                for c in range(16):
                    P.op("pe", lambda e, c=c: e.matmul(
                        ps[:, b4 * 512 + c * 128:b4 * 512 + (c + 1) * 128],
                        lhsT=xacc[:, ti, c * 128:(c + 1) * 128], rhs=dgr[:, :], start=True, stop=True),
                         reads=[t_xa[ti], tdgr], writes=[t_pb[b4 + c // 4]])

            def n2_N3(ti):
                b4 = n2.pop(ti)
                hf, thf = h2Tf[ti % 2], t_h2Tf[ti % 2]
                for c in range(16):
                    src = ps[:, b4 * 512 + c * 128:b4 * 512 + (c + 1) * 128]
                    if c % 4 != 3:
                        P.op("act", lambda e, c=c, src=src: e.activation(
                            out=hf[:, c, :], in_=src, func=AF.Identity, scale=a12[:, 16 + c:17 + c],
                            bias=ada_fm[:, 48 + c:49 + c]),
                             reads=[t_pb[b4 + c // 4], t_a2, t_ada2], writes=[thf], same_ok=True)
                    else:
                        P.op("dve", lambda e, c=c, src=src: e.tensor_scalar(
                            out=hf[:, c, :], in0=src, scalar1=a12[:, 16 + c:17 + c],
                            scalar2=ada_fm[:, 48 + c:49 + c], op0=ALU.mult, op1=ALU.add),
                             reads=[t_pb[b4 + c // 4], t_a2, t_ada2], writes=[thf], same_ok=True)
                bfree(b4, 4)
                P.op("pool", lambda e: e.tensor_copy(out=h2T[:, :, ti * 128:(ti + 1) * 128], in_=hf),
                     reads=[thf], writes=[t_h2T[ti]])

            def n2_R(ti):
                hf, thf = h2Tf[ti % 2], t_h2Tf[ti % 2]
                rtb, trt = rts[ti % 2], t_rts[ti % 2]
                br = bank()
                wr3 = cs(C_WR, 320).rearrange(K3, k=16)
                mm(pbank(br, 1, 0, 20), [(hf[:, k, :], wr3[:, k, :]) for k in range(16)], [thf, t_cst], [t_pb[br]])
                P.op("dve", lambda e: e.tensor_tensor(out=rtb[:, 0:20], in0=pbank(br, 1, 0, 20), in1=cs(C_BR, 20),
                                                     op=ALU.add), reads=[trt, t_pb[br], t_cst], writes=[trt])
                bfree(br)

            def router_ops(ti):
                rtb, trt = rts[ti % 2], t_rts[ti % 2]
                R = lambda a_, b_: rtb[:, a_:b_]
                ops = []
                ro = lambda fn, rd=(): ops.append(lambda: P.op("dve", fn, reads=[trt] + list(rd), writes=[trt]))
                ra = lambda fn: ops.append(lambda: P.op("act", fn, reads=[trt], writes=[trt]))

                ro(lambda e: e.tensor_reduce(out=R(20, 21), in_=R(0, 4), axis=AX.X, op=ALU.max))
                ro(lambda e: e.tensor_scalar(out=R(21, 22), in0=R(20, 21), scalar1=-1.0, scalar2=None, op0=ALU.mult))
                ra(lambda e: e.activation(out=R(24, 28), in_=R(0, 4), func=AF.Exp, bias=R(21, 22), scale=1.0,
                                          accum_out=R(22, 23)))
                ro(lambda e: e.reciprocal(out=R(23, 24), in_=R(22, 23)))
                ro(lambda e: e.tensor_scalar(out=R(28, 32), in0=R(0, 4), scalar1=R(20, 21), scalar2=None,
                                             op0=ALU.is_equal))
                ro(lambda e: e.tensor_tensor(out=R(32, 48).rearrange("p (g e) -> p g e", g=4),
                                             in0=R(4, 20).rearrange("p (g e) -> p g e", g=4),
                                             in1=R(28, 32).unsqueeze(2).broadcast_to([128, 4, 4]), op=ALU.mult))
                ro(lambda e: e.tensor_reduce(out=R(48, 52), in_=R(32, 48).rearrange("p (g e) -> p e g", g=4),
                                             axis=AX.X, op=ALU.add))
                ro(lambda e: e.tensor_reduce(out=R(52, 53), in_=R(48, 52), axis=AX.X, op=ALU.max))
                ro(lambda e: e.tensor_scalar(out=R(56, 60), in0=R(48, 52), scalar1=R(52, 53), scalar2=None,
                                             op0=ALU.is_equal))
                ro(lambda e: e.scalar_tensor_tensor(out=R(60, 64), in0=R(56, 60), scalar=NEG, in1=R(48, 52),
                                                    op0=ALU.mult, op1=ALU.add))
                ro(lambda e: e.tensor_reduce(out=R(53, 54), in_=R(60, 64), axis=AX.X, op=ALU.max))
                ro(lambda e: e.tensor_scalar(out=R(64, 68), in0=R(60, 64), scalar1=R(53, 54), scalar2=None,
                                             op0=ALU.is_equal))
                ro(lambda e: e.tensor_tensor(out=R(54, 55), in0=R(53, 54), in1=R(52, 53), op=ALU.subtract))
                ra(lambda e: e.activation(out=R(55, 56), in_=R(54, 55), func=AF.Exp))
                ro(lambda e: e.tensor_scalar(out=R(68, 69), in0=R(55, 56), scalar1=1.0, scalar2=None, op0=ALU.add))
                ro(lambda e: e.reciprocal(out=R(69, 70), in_=R(68, 69)))
                ro(lambda e: e.tensor_tensor(out=R(70, 71), in0=R(55, 56), in1=R(69, 70), op=ALU.mult))
                ro(lambda e: e.tensor_tensor(out=R(71, 72), in0=R(69, 70), in1=R(23, 24), op=ALU.mult))
                ro(lambda e: e.tensor_tensor(out=R(72, 73), in0=R(70, 71), in1=R(23, 24), op=ALU.mult))
                ro(lambda e: e.tensor_scalar(out=R(76, 80), in0=R(56, 60), scalar1=R(71, 72), scalar2=None,
                                             op0=ALU.mult))
                ro(lambda e: e.scalar_tensor_tensor(out=R(80, 84), in0=R(64, 68), scalar=R(72, 73), in1=R(76, 80),
                                                    op0=ALU.mult, op1=ALU.add))
                ops.append(lambda: P.op("dve", lambda e: e.tensor_tensor(
                    out=comb[:, ti, :].rearrange("p (g e) -> p g e", g=4),
                    in0=R(28, 32).unsqueeze(2).broadcast_to([128, 4, 4]),
                    in1=R(80, 84).unsqueeze(1).broadcast_to([128, 4, 4]), op=ALU.mult),
                    reads=[trt], writes=[t_comb], same_ok=True))
                return ops

            def router_pair(t0_, t1_):
                la, lb = router_ops(t0_), router_ops(t1_)
                for fa, fb in zip(la, lb):
                    fa()
                    fb()

            n2_N1(0)
            n2_N1(1)
            n2_N2(0)
            n2_N2(1)
            n2_N1(2)
            n2_N1(3)
            n2_N3(0)
            n2_R(0)
            n2_N2(2)
            n2_N3(1)
            n2_R(1)
            router_pair(0, 1)
            n2_N2(3)
            n2_N3(2)
            n2_R(2)
            n2_N3(3)
            n2_R(3)
            router_pair(2, 3)

            regen_bc(ada_fm[:, 80:96], [t_ada2])
            kst = 0
            for ex in range(NEXP):
                if ex == HOIST_AT:
                    P.op("sp", lambda e: e.dma_start(out=bc2, in_=fg_d), writes=[t_bc2], dma="bc2")
                    if blk + 1 < NB:
                        P.fence(t_xnj + t_xt, t_xn2j + t_h2Tf)
                        hoist_stage = emit_M1a((blk + 1) * TB)
                if blk + 1 < NB and HOIST_AT <= ex < HOIST_AT + 6:
                    hoist_stage(ex - HOIST_AT)
                ab, tab = actb[ex % 2], t_act[ex % 2]
                for hh in range(2):
                    sg_ = wnext("eg")
                    su_ = wnext("eu")
                    wg3, wu3 = slot3(sg_, 16), slot3(su_, 16)
                    for ff in range(2):
                        fc = hh * 2 + ff
                        bg = bank()
                        mm(pbank(bg), [(wg3[:, k, ff * 128:(ff + 1) * 128], h2T[:, k, :]) for k in range(16)],
                           [t_w[sg_]] + t_h2T, [t_pb[bg]])
                        bu = bank()
                        mm(pbank(bu), [(wu3[:, k, ff * 128:(ff + 1) * 128], h2T[:, k, :]) for k in range(16)],
                           [t_w[su_]] + t_h2T, [t_pb[bu]])
                        i2 = fc % 2
                        P.op("act", lambda e, bg=bg, i2=i2: e.activation(out=sl[i2], in_=pbank(bg), func=AF.Silu),
                             reads=[t_pb[bg]], writes=[t_sl[i2]])
                        P.op("dve", lambda e, bu=bu, i2=i2, ab=ab, fc=fc: e.tensor_tensor(out=ab[:, fc, :], in0=sl[i2],
                                                                                         in1=pbank(bu), op=ALU.mult),
                             reads=[t_sl[i2], t_pb[bu]], writes=[tab], same_ok=True)
                        bfree(bg)
                        bfree(bu)
                    wrel(2)
                for dh in range(2):
                    sd_ = wnext("ed")
                    wd3 = slot3(sd_, 4)
                    for ti in range(4):
                        for dd in range(2):
                            db = dh * 2 + dd
                            b = bank()
                            mm(pbank(b), [(ab[:, fc, ti * 128:(ti + 1) * 128], wd3[:, fc, dd * 512:(dd + 1) * 512])
                                          for fc in range(4)], [t_w[sd_], tab], [t_pb[b]])
                            i4 = kst % 4
                            kst += 1
                            P.op("dve", lambda e, b=b, ti=ti, ex=ex, db=db, i4=i4: e.scalar_tensor_tensor(
                                out=stmp[i4], in0=pbank(b), scalar=comb[:, ti, ex:ex + 1],
                                in1=bc[:, db * 512:(db + 1) * 512], op0=ALU.mult, op1=ALU.mult),
                                 reads=[t_pb[b], t_comb, t_bc], writes=[t_stmp[i4]])
                            bfree(b)
                            P.op("pool", lambda e, ti=ti, db=db, i4=i4: e.tensor_tensor(
                                out=xacc[:, ti, db * 512:(db + 1) * 512], in0=xacc[:, ti, db * 512:(db + 1) * 512],
                                in1=stmp[i4], op=ALU.add),
                                 reads=[t_stmp[i4]], writes=[t_xa[ti]], same_ok=True)
                    wrel()

            for ti in range(4):
                rstd, g = rms_stats(xacc[:, ti, :], [t_xa[ti]], fjunk, [t_fj], 6 + ti % 2)
                P.op("dve", lambda e, ti=ti, rstd=rstd: e.scalar_tensor_tensor(
                    out=xacc[:, ti, :], in0=xacc[:, ti, :], scalar=rstd, in1=bc2, op0=ALU.mult, op1=ALU.mult),
                     reads=[g, t_bc2], writes=[t_xa[ti]])
                P.op("sp", lambda e, ti=ti, r0=r0: e.dma_start(out=y_d[r0 + ti * 128:r0 + (ti + 1) * 128, :],
                                                               in_=xacc[:, ti, :]),
                     reads=[t_xa[ti]], dma="out%d" % ti)

        assert wq["acq"] == len(specs) and wq["released"] == len(specs), (wq, len(specs))
        P.emit(nc, sems, dsem, ["out%d" % i for i in range(4)])
    return nc


_NC_CACHE = {}


def _host_consts(core, c, b_ada, norm1_g, sinks, pool_scale, norm2_g, w_router_group, b_router_group,
                 w_router_expert, b_router_expert, final_g):
    b, q = core // 4, core % 4
    fm = lambda v: np.ascontiguousarray(v.reshape(-1, 128).T)
    cst = np.zeros((128, C_TOT), np.float32)
    cst[:, C_C:C_C + 16] = fm(c[b])
    cst[:, C_BADA:C_BADA + 96] = fm(b_ada[0])
    cst[:, C_G1:C_G1 + 16] = fm(norm1_g[0])
    cst[:, C_SINK:C_SINK + 16] = sinks[0][None, :]
    cst[:, C_PSC:C_PSC + 8] = fm(pool_scale[0])
    cst[:, C_G2:C_G2 + 16] = fm(norm2_g[0])
    cst[:, C_BR:C_BR + 4] = b_router_group[0][None, :]
    cst[:, C_BR + 4:C_BR + 20] = b_router_expert[0][None, :]
    cst[:, C_FG:C_FG + 16] = fm(final_g)
    cst[:, C_UF] = 0.0 if q == 0 else 1.0
    for g, w in enumerate((2, 4, 8, 16)):
        t = np.arange(16)
        cst[:, C_INVC + g * 16:C_INVC + (g + 1) * 16] = (1.0 / np.minimum(t + 1, w) if q == 0
                                                         else np.full(16, 1.0 / w))[None, :]
    r = np.arange(128)[:, None]
    j = np.arange(256)[None, :]
    valid = (j > r) & (j <= r + 128)
    am = np.where(valid, 0.0, NEG).astype(np.float32)
    cst[:, C_AM:C_AM + 256] = am
    am0 = am.copy()
    if q == 0:
        am0[:, :128] = NEG
    cst[:, C_AM0:C_AM0 + 256] = am0
    wr = np.concatenate([w_router_group[0], w_router_expert[0]], axis=1)
    cst[:, C_WR:C_WR + 320] = wr.reshape(16, 128, 20).transpose(1, 0, 2).reshape(128, 320)
    return cst


def kernel(x, c, w_ada, b_ada, norm1_g, w_in, sinks, w_pool, pool_scale, w_attn_branch, w_pool_branch, w_out,
           norm2_g, w_router_group, b_router_group, w_router_expert, b_router_expert, w_e_gate, w_e_up,
           w_e_down, final_g):
    f = lambda a: np.ascontiguousarray(np.asarray(a, dtype=np.float32))
    x = f(x)
    if "nc" not in _NC_CACHE:
        _NC_CACHE["nc"] = build_nc()
    nc = _NC_CACHE["nc"]
    shared = {
        "w_ada": f(w_ada)[0], "w_in": f(w_in)[0], "w_pool": f(w_pool)[0], "w_ab": f(w_attn_branch)[0],
        "w_pb": f(w_pool_branch)[0], "w_out": f(w_out)[0], "w_eg": f(w_e_gate)[0], "w_eu": f(w_e_up)[0],
        "w_ed": f(w_e_down)[0],
    }
    small = [np.asarray(a, dtype=np.float32) for a in (c, b_ada, norm1_g, sinks, pool_scale, norm2_g,
                                                        w_router_group, b_router_group, w_router_expert,
                                                        b_router_expert, final_g)]
    fg_bc = np.ascontiguousarray(np.broadcast_to(np.asarray(final_g, dtype=np.float32)[None, :], (128, D)))
    in_maps = []
    for core in range(NCORE):
        b, q = core // 4, core % 4
        xc = np.zeros((NTOK + 128, D), np.float32)
        if q > 0:
            xc[:128] = x[b, q * NTOK - 128:q * NTOK]
        xc[128:] = x[b, q * NTOK:(q + 1) * NTOK]
        m = dict(shared)
        m["x"] = xc
        m["cst"] = _host_consts(core, *small)
        m["fg_bc"] = fg_bc
        in_maps.append(m)
    res = run_bass_kernel_spmd(nc, in_maps, core_ids=list(range(NCORE)))
    out = np.empty((2, 4 * NTOK, D), np.float32)
    for core in range(NCORE):
        b, q = core // 4, core % 4
        out[b, q * NTOK:(q + 1) * NTOK] = res.results[core]["y"]
    return out
```
